# Optimizing a Trainium2 kernel written in Bass

```python
import math
import jax, jax.numpy as jnp
from jax import lax
import numpy as np


D_MODEL = 2048
BATCH = 1
SEQ = 8192
DEPTH = 1

CTX_LEN = 256
GRID_W = 64
HEAD_DIM = 128
ROPE_PAIRS = HEAD_DIM // 4
ROPE_THETA = 10000.0
A_HEADS = 8
A_KV_HEADS = 2
A_GROUP = A_HEADS // A_KV_HEADS
B_HEADS = 4
B_V_DIM = 2 * HEAD_DIM
N_EXPERTS = 64
TOP_K = 8
N_GROUPS = 8
TOPK_GROUPS = 4
EXPERT_DIM = 512
SHARED_DIM = 512
ROUTED_SCALE = 2.5
EXPERT_BLOCK = 128
Q_BLOCK = 128
N_MOD = 6
EPS = 1e-6

A_Q_W = A_HEADS * HEAD_DIM
A_KV_W = A_KV_HEADS * HEAD_DIM
B_QK_W = B_HEADS * 2 * HEAD_DIM
B_V_W = B_HEADS * B_V_DIM
IN_SIZES = (A_Q_W, A_KV_W, A_KV_W, B_QK_W, B_QK_W, B_V_W, D_MODEL, D_MODEL)
IN_W = sum(IN_SIZES)
IN_OFFSETS = tuple(sum(IN_SIZES[:j + 1]) for j in range(len(IN_SIZES) - 1))

kernel_name = 'hybrid_gated_gqa_diffattn_moe_dit'


def rms_norm(x, g):
    xf = x.astype(jnp.float32)
    y = xf * lax.rsqrt(jnp.mean(xf * xf, axis=-1, keepdims=True) + EPS)
    return (y * g.astype(jnp.float32)).astype(x.dtype)


def adaln(cvec, w, b):
    m = jax.nn.silu(cvec) @ w + b
    return m.reshape(cvec.shape[0], N_MOD, 1, D_MODEL)


def modulate(h, shift, scale):
    return h * (1 + scale) + shift


def rope_tables(rows_n):
    rows = jnp.repeat(jnp.arange(rows_n, dtype=jnp.float32), GRID_W)
    cols = jnp.tile(jnp.arange(GRID_W, dtype=jnp.float32), rows_n)
    inv = ROPE_THETA ** (-jnp.arange(ROPE_PAIRS, dtype=jnp.float32) / ROPE_PAIRS)
    ang_r = rows[:, None] * inv
    ang_c = cols[:, None] * inv
    return (jnp.cos(ang_r)[:, None, :], jnp.sin(ang_r)[:, None, :],
            jnp.cos(ang_c)[:, None, :], jnp.sin(ang_c)[:, None, :])


def _rotate(u, cos, sin):
    u1, u2 = jnp.split(u, 2, axis=-1)
    return jnp.concatenate([u1 * cos - u2 * sin, u1 * sin + u2 * cos], axis=-1)


def apply_rope(x, tables):
    cr, sr, cc, sc = (t.astype(x.dtype) for t in tables)
    half = HEAD_DIM // 2
    return jnp.concatenate([_rotate(x[..., :half], cr, sr),
                            _rotate(x[..., half:], cc, sc)], axis=-1)


def project_heads(h, w_in, qn_a, kn_a, qn_b, kn_b, tables):
    b_, t_, _ = h.shape
    aq, ak, av, bq, bk, bv, ga, gb = jnp.split(h @ w_in, IN_OFFSETS, axis=-1)
    aq = rms_norm(aq.reshape(b_, t_, A_HEADS, HEAD_DIM), qn_a)
    ak = rms_norm(ak.reshape(b_, t_, A_KV_HEADS, HEAD_DIM), kn_a)
    bq = rms_norm(bq.reshape(b_, t_, 2 * B_HEADS, HEAD_DIM), qn_b)
    bk = rms_norm(bk.reshape(b_, t_, 2 * B_HEADS, HEAD_DIM), kn_b)
    if tables is not None:
        aq = apply_rope(aq, tables)
        ak = apply_rope(ak, tables)
        bq = apply_rope(bq, tables)
        bk = apply_rope(bk, tables)
    aq = aq.reshape(b_, t_, A_KV_HEADS, A_GROUP, HEAD_DIM)
    av = av.reshape(b_, t_, A_KV_HEADS, HEAD_DIM)
    bq = bq.reshape(b_, t_, B_HEADS, 2, HEAD_DIM)
    bk = bk.reshape(b_, t_, B_HEADS, 2, HEAD_DIM)
    bv = bv.reshape(b_, t_, B_HEADS, B_V_DIM)
    return aq, ak, av, bq, bk, bv, ga, gb


def gqa_core(q, k, v):
    s = jnp.einsum('bqkgd,bskd->bkgqs', q, k, preferred_element_type=jnp.float32)
    p = jax.nn.softmax(s * (HEAD_DIM ** -0.5), axis=-1)
    return jnp.einsum('bkgqs,bskd->bqkgd', p.astype(v.dtype), v)


def diff_core(q, k, v, lam):
    s = jnp.einsum('bqhmd,bshmd->bhmqs', q, k, preferred_element_type=jnp.float32)
    p = jax.nn.softmax(s * (HEAD_DIM ** -0.5), axis=-1)
    a = p[:, :, 0] - lam * p[:, :, 1]
    return jnp.einsum('bhqs,bshe->bqhe', a.astype(v.dtype), v)


def _to_blocks(a):
    b_, t_ = a.shape[:2]
    a = a.reshape((b_, t_ // Q_BLOCK, Q_BLOCK) + a.shape[2:])
    return jnp.moveaxis(a, 1, 0)


def _from_blocks(a):
    a = jnp.moveaxis(a, 0, 1)
    return a.reshape((a.shape[0], a.shape[1] * a.shape[2]) + a.shape[3:])


def sweep_latent(core, q):
    return _from_blocks(lax.map(core, _to_blocks(q)))


def merge_branches(oa, ob, ga, gb, subln_g, lam_init, wa, wb, wo):
    b_, t_ = oa.shape[:2]
    oa = oa.reshape(b_, t_, A_Q_W)
    ob = (rms_norm(ob, subln_g) * (1.0 - lam_init)).reshape(b_, t_, B_V_W)
    y = jax.nn.sigmoid(ga) * (oa @ wa) + jax.nn.sigmoid(gb) * (ob @ wb)
    return y @ wo


def swiglu(x, wg, wu, wd):
    return (jax.nn.silu(x @ wg) * (x @ wu)) @ wd


def routed_experts(hf, idx, wsel, weg, weu, wed):
    n = hf.shape[0]
    n_assign = n * TOP_K
    flat_e = idx.reshape(-1)
    order = jnp.argsort(flat_e, stable=True)
    sorted_e = flat_e[order]
    sorted_tok = (order // TOP_K).astype(jnp.int32)
    sorted_w = wsel.reshape(-1)[order]
    counts = jnp.bincount(flat_e, length=N_EXPERTS)
    padded = (counts + EXPERT_BLOCK - 1) // EXPERT_BLOCK * EXPERT_BLOCK
    pad_end = jnp.cumsum(padded)
    pad_start = pad_end - padded
    start = jnp.cumsum(counts) - counts
    dest = pad_start[sorted_e] + (jnp.arange(n_assign) - start[sorted_e])
    n_blocks = -(-n_assign // EXPERT_BLOCK) + N_EXPERTS
    total = n_blocks * EXPERT_BLOCK
    tok_buf = jnp.zeros((total,), jnp.int32).at[dest].set(sorted_tok)
    w_buf = jnp.zeros((total,), hf.dtype).at[dest].set(sorted_w)
    block_e = jnp.minimum(
        jnp.searchsorted(pad_end, jnp.arange(n_blocks) * EXPERT_BLOCK, side='right'),
        N_EXPERTS - 1)

    def step(out, blk):
        tok, wt, e = blk
        y = swiglu(hf[tok], weg[e], weu[e], wed[e]) * wt[:, None]
        return out.at[tok].add(y), None

    out, _ = lax.scan(step, jnp.zeros_like(hf),
                      (tok_buf.reshape(n_blocks, EXPERT_BLOCK),
                       w_buf.reshape(n_blocks, EXPERT_BLOCK), block_e))
    return out


def moe(h, w_router, router_bias, weg, weu, wed, wsg, wsu, wsd):
    shape = h.shape
    hf = h.reshape(-1, D_MODEL)
    n = hf.shape[0]
    scores = jax.nn.sigmoid((hf @ w_router).astype(jnp.float32))
    biased = scores + router_bias.astype(jnp.float32)
    per_group = N_EXPERTS // N_GROUPS
    grp_score = lax.top_k(biased.reshape(n, N_GROUPS, per_group), 2)[0].sum(-1)
    _, top_groups = lax.top_k(grp_score, TOPK_GROUPS)
    group_mask = jax.nn.one_hot(top_groups, N_GROUPS, dtype=jnp.float32).sum(1) > 0
    expert_mask = jnp.repeat(group_mask, per_group, axis=1)
    _, idx = lax.top_k(jnp.where(expert_mask, biased, -jnp.inf), TOP_K)
    wsel = jnp.take_along_axis(scores, idx, axis=1)
    wsel = wsel / jnp.sum(wsel, axis=-1, keepdims=True) * ROUTED_SCALE
    routed = routed_experts(hf, idx, wsel.astype(hf.dtype), weg, weu, wed)
    return (routed + swiglu(hf, wsg, wsu, wsd)).reshape(shape)


def setup_inputs(seed: int = 0) -> dict:
    key = jax.random.key(seed)
    ks = jax.random.split(key, 29)
    f32 = jnp.float32

    def nrm(k, shape, scale):
        return jax.random.normal(k, shape, f32) * scale

    def gain(k, shape):
        return 1.0 + 0.02 * jax.random.normal(k, shape, f32)

    D, E, F, FS = D_MODEL, N_EXPERTS, EXPERT_DIM, SHARED_DIM
    return {
        'x': nrm(ks[0], (BATCH, SEQ, D), 1.0),
        'c': nrm(ks[1], (BATCH, D), 1.0),
        'ctx': nrm(ks[2], (BATCH, CTX_LEN, D), 1.0),
        'c_ctx': nrm(ks[3], (D,), 1.0),
        'w_ada': nrm(ks[4], (DEPTH, D, N_MOD * D), 0.5 * D ** -0.5),
        'b_ada': nrm(ks[5], (DEPTH, N_MOD * D), 0.02),
        'norm_mix': gain(ks[6], (DEPTH, D)),
        'norm_ffn': gain(ks[7], (DEPTH, D)),
        'w_in': nrm(ks[8], (DEPTH, D, IN_W), D ** -0.5),
        'q_norm_a': gain(ks[9], (DEPTH, HEAD_DIM)),
        'k_norm_a': gain(ks[10], (DEPTH, HEAD_DIM)),
        'q_norm_b': gain(ks[11], (DEPTH, HEAD_DIM)),
        'k_norm_b': gain(ks[12], (DEPTH, HEAD_DIM)),
        'lambda_q1': nrm(ks[13], (DEPTH, HEAD_DIM), 0.1),
        'lambda_k1': nrm(ks[14], (DEPTH, HEAD_DIM), 0.1),
        'lambda_q2': nrm(ks[15], (DEPTH, HEAD_DIM), 0.1),
        'lambda_k2': nrm(ks[16], (DEPTH, HEAD_DIM), 0.1),
        'subln_b': gain(ks[17], (DEPTH, B_V_DIM)),
        'w_branch_a': nrm(ks[18], (DEPTH, A_Q_W, D), A_Q_W ** -0.5),
        'w_branch_b': nrm(ks[19], (DEPTH, B_V_W, D), B_V_W ** -0.5),
        'w_out': nrm(ks[20], (DEPTH, D, D), D ** -0.5),
        'w_router': nrm(ks[21], (DEPTH, D, E), D ** -0.5),
        'router_bias': nrm(ks[22], (DEPTH, E), 0.01),
        'w_exp_gate': nrm(ks[23], (DEPTH, E, D, F), D ** -0.5),
        'w_exp_up': nrm(ks[24], (DEPTH, E, D, F), D ** -0.5),
        'w_exp_down': nrm(ks[25], (DEPTH, E, F, D), F ** -0.5),
        'w_sh_gate': nrm(ks[26], (DEPTH, D, FS), D ** -0.5),
        'w_sh_up': nrm(ks[27], (DEPTH, D, FS), D ** -0.5),
        'w_sh_down': nrm(ks[28], (DEPTH, FS, D), FS ** -0.5),
    }


def reference(x, c, ctx, c_ctx, w_ada, b_ada, norm_mix, norm_ffn, w_in,
              q_norm_a, k_norm_a, q_norm_b, k_norm_b,
              lambda_q1, lambda_k1, lambda_q2, lambda_k2, subln_b,
              w_branch_a, w_branch_b, w_out, w_router, router_bias,
              w_exp_gate, w_exp_up, w_exp_down, w_sh_gate, w_sh_up, w_sh_down):
    ROWS = x.shape[1] // GRID_W
    tables = rope_tables(ROWS)
    f32 = jnp.float32
    for i in range(DEPTH):
        last = i == DEPTH - 1
        lam_init = 0.8 - 0.6 * math.exp(-0.3 * i)
        lam = (jnp.exp(jnp.sum(lambda_q1[i].astype(f32) * lambda_k1[i].astype(f32)))
               - jnp.exp(jnp.sum(lambda_q2[i].astype(f32) * lambda_k2[i].astype(f32)))
               + lam_init)
        mod = adaln(c, w_ada[i], b_ada[i])
        mod_c = adaln(c_ctx[None, :], w_ada[i], b_ada[i])

        h = modulate(rms_norm(x, norm_mix[i]), mod[:, 0], mod[:, 1])
        hc = modulate(rms_norm(ctx, norm_mix[i]), mod_c[:, 0], mod_c[:, 1])
        aq, ak, av, bq, bk, bv, ga, gb = project_heads(
            h, w_in[i], q_norm_a[i], k_norm_a[i], q_norm_b[i], k_norm_b[i], tables)
        caq, cak, cav, cbq, cbk, cbv, cga, cgb = project_heads(
            hc, w_in[i], q_norm_a[i], k_norm_a[i], q_norm_b[i], k_norm_b[i], None)
        ak_all = jnp.concatenate([cak, ak], axis=1)
        av_all = jnp.concatenate([cav, av], axis=1)
        bk_all = jnp.concatenate([cbk, bk], axis=1)
        bv_all = jnp.concatenate([cbv, bv], axis=1)
        oa = sweep_latent(lambda qb: gqa_core(qb, ak_all, av_all), aq)
        ob = sweep_latent(lambda qb: diff_core(qb, bk_all, bv_all, lam), bq)
        y = merge_branches(oa, ob, ga, gb, subln_b[i], lam_init,
                           w_branch_a[i], w_branch_b[i], w_out[i])
        x = x + mod[:, 2] * y

        if not last:
            oac = gqa_core(caq, cak, cav)
            obc = diff_core(cbq, cbk, cbv, lam)
            yc = merge_branches(oac, obc, cga, cgb, subln_b[i], lam_init,
                                w_branch_a[i], w_branch_b[i], w_out[i])
            ctx = ctx + mod_c[:, 2] * yc
            hc2 = modulate(rms_norm(ctx, norm_ffn[i]), mod_c[:, 3], mod_c[:, 4])
            ctx = ctx + mod_c[:, 5] * moe(hc2, w_router[i], router_bias[i],
                                          w_exp_gate[i], w_exp_up[i], w_exp_down[i],
                                          w_sh_gate[i], w_sh_up[i], w_sh_down[i])

        h2 = modulate(rms_norm(x, norm_ffn[i]), mod[:, 3], mod[:, 4])
        x = x + mod[:, 5] * moe(h2, w_router[i], router_bias[i],
                                w_exp_gate[i], w_exp_up[i], w_exp_down[i],
                                w_sh_gate[i], w_sh_up[i], w_sh_down[i])
    return x
```

```python
import os
import numpy as np
import concourse.bass as bass
import concourse.mybir as mybir
from concourse.bass_utils import run_bass_kernel_spmd

F32 = mybir.dt.float32
BF16 = mybir.dt.bfloat16
I32 = mybir.dt.int32
ALU = mybir.AluOpType
AF = mybir.ActivationFunctionType
AX = mybir.AxisListType

D = 2048
NCORE = 8
TOK = 1024
NTT = TOK // 128
SEQ = 8192
CTX = 256
NKEY = SEQ + CTX
NST = NKEY // 128
EPS = 1e-6
INW = 8704
NEXP = 64
FE = 512
SCALE = 128 ** -0.5
CAPS = [((min(1024, 8192 // (r + 1)) + 63) // 64) * 64 for r in range(NEXP)]
BASES = [sum(CAPS[:r]) for r in range(NEXP)]
TRASH = sum(CAPS)
NSLOT = TRASH
LAM_INIT = 0.2
ENG = ('pe', 'act', 'dve', 'pool', 'sp')
SEM_LIMIT = 8000


class Res:
    __slots__ = ('name', 'w', 'r')

    def __init__(self, name):
        self.name = name
        self.w = {}
        self.r = {}


class Op:
    pass


class Sched:
    def __init__(self, nc):
        self.nc = nc
        self.ops = {e: [] for e in ENG}
        self.waited = {e: {} for e in ENG}
        self.chans = {}

    def _need(self, eng, toks):
        waits = []
        for tok in toks:
            if tok[0] == 'e':
                _, peng, op = tok
                if peng == 'pe' and eng == 'pe':
                    continue
                key = ('e', peng)
                if self.waited[eng].get(key, -1) >= op.idx:
                    continue
                self.waited[eng][key] = op.idx
                op.marked = True
                waits.append(tok)
            else:
                _, ch, val = tok
                key = ('d', ch)
                if self.waited[eng].get(key, 0) >= val:
                    continue
                self.waited[eng][key] = val
                waits.append(tok)
        return waits

    def _deps(self, reads, writes):
        toks = []
        for r in reads:
            toks += list(r.w.values())
        for w in writes:
            toks += list(w.w.values()) + list(w.r.values())
        return toks

    def op(self, eng, fn, reads=(), writes=()):
        o = Op()
        o.eng = eng
        o.fn = fn
        o.idx = len(self.ops[eng])
        o.marked = False
        o.dma = None
        o.waits = self._need(eng, self._deps(reads, writes))
        self.ops[eng].append(o)
        tok = ('e', eng, o)
        for r in reads:
            r.r[('e', eng)] = tok
        for w in writes:
            w.w[('e', eng)] = tok
            w.r = {}
        return o

    def dma(self, q, fn, reads=(), writes=(), chan=None, accw=()):
        ch = self.chans.setdefault(chan, [None, 0])
        ch[1] += 16
        val = ch[1]
        o = Op()
        o.eng = q
        o.fn = fn
        o.idx = len(self.ops[q])
        o.marked = False
        o.dma = (chan, val)
        toks = self._deps(reads, writes)
        for w in accw:
            toks += list(w.r.values())
        o.waits = self._need(q, toks)
        self.ops[q].append(o)
        tok = ('d', chan, val)
        for r in reads:
            r.r[('d', chan)] = tok
        for w in writes:
            w.w[('d', chan)] = tok
            w.r = {}
        for w in accw:
            w.w[('d', chan)] = tok
        return o

    def barrier(self):
        last = {}
        for e in ENG:
            for o in reversed(self.ops[e]):
                if o.dma is None and o.fn is not None:
                    last[e] = o
                    break
        for e in ENG:
            toks = [('e', pe, o) for pe, o in last.items() if not (pe == e and e in ('pe', 'sp'))]
            toks += [('d', ch, v[1]) for ch, v in self.chans.items()]
            w = self._need(e, toks)
            if w:
                o = Op()
                o.eng = e
                o.fn = None
                o.idx = len(self.ops[e])
                o.marked = False
                o.dma = None
                o.waits = w
                self.ops[e].append(o)

    def emit(self):
        nc = self.nc
        nsem = 0
        for e in ENG:
            cnt = 0
            sem = None
            for o in self.ops[e]:
                if o.marked:
                    if sem is None or cnt >= SEM_LIMIT:
                        sem = nc.alloc_semaphore(f"pg_{e}_{nsem}")
                        nsem += 1
                        cnt = 0
                    cnt += 1
                    o.sem = sem
                    o.val = cnt
        for i, (ch, v) in enumerate(self.chans.items()):
            v[0] = nc.alloc_semaphore(f"ch_{i}")
        sched = self

        def run(engname):
            def body(e):
                for o in sched.ops[engname]:
                    for tok in o.waits:
                        if tok[0] == 'e':
                            e.wait_ge(tok[2].sem, tok[2].val)
                        else:
                            e.wait_ge(sched.chans[tok[1]][0], tok[2])
                    if o.fn is None:
                        continue
                    ins = o.fn(e)
                    if o.dma is not None:
                        ins.then_inc(sched.chans[o.dma[0]][0], 16)
                    elif o.marked:
                        ins.then_inc(o.sem, 1)
                if engname == 'sp':
                    for ch, v in sched.chans.items():
                        if v[1] > 0:
                            e.wait_ge(v[0], v[1])
            return body

        with nc.Block() as block:
            block.tensor(run('pe'))
            block.scalar(run('act'))
            block.vector(run('dve'))
            block.gpsimd(run('pool'))
            block.sync(run('sp'))


DBG_OFFS = {}


class Arena:
    def __init__(self, nc, nbytes):
        self.n2 = nbytes // 2
        self.base = nc.alloc_sbuf_tensor("arena", [128, self.n2], BF16)
        self.top = 0
        self.limit = self.n2

    def alloc(self, shape, dtype, name):
        P = shape[0]
        n = 1
        for s in shape[1:]:
            n *= s
        esz = 4 if dtype in (F32, I32) else 2
        n2 = (n * esz + 1) // 2
        n2 = (n2 + 15) // 16 * 16
        off = self.top
        self.top += n2
        DBG_OFFS[name] = (off, list(shape), str(dtype))
        assert self.top <= self.limit, f"SBUF arena overflow at {name}: {self.top * 2} > {self.limit * 2}"
        ap = self.base[0:P, off:off + (n * esz) // 2]
        if dtype != BF16:
            ap = ap.bitcast(dtype)
        if len(shape) == 3:
            ap = ap.rearrange("p (a b) -> p a b", a=shape[1])
        elif len(shape) == 4:
            ap = ap.rearrange("p (a b c) -> p a b c", a=shape[1], b=shape[2])
        return ap, Res(name)


def bfv(ap):
    v = ap.bitcast(BF16)
    if len(ap.shape) == 2:
        return v.rearrange("p (n two) -> p n two", two=2)[:, :, 1]
    return v.rearrange("p k (n two) -> p k n two", two=2)[:, :, :, 1]


def build_program():
    nc = bass.Bass("TRN2", target_bir_lowering=False)
    S = Sched(nc)

    def din(name, shape, dt=F32):
        return nc.dram_tensor(name, shape, dt, kind="ExternalInput").ap()

    x_all = din("x_all", [SEQ, D])
    x_own = din("x_own", [TOK, D])
    ctx_in = din("ctx", [CTX, D])
    c_in = din("c", [D])
    cctx_in = din("c_ctx", [D])
    w_ada = din("w_ada", [D, 6 * D])
    b_ada = din("b_ada", [6 * D])
    norm_mix = din("norm_mix", [D])
    norm_ffn = din("norm_ffn", [D])
    w_in = din("w_in", [D, INW])
    qn_a = din("q_norm_a", [128])
    kn_a = din("k_norm_a", [128])
    qn_b = din("q_norm_b", [128])
    kn_b = din("k_norm_b", [128])
    lq1 = din("lambda_q1", [128])
    lk1 = din("lambda_k1", [128])
    lq2 = din("lambda_q2", [128])
    lk2 = din("lambda_k2", [128])
    subln = din("subln_b", [256])
    w_ba = din("w_branch_a", [1024, D])
    w_bb = din("w_branch_b", [1024, D])
    w_out = din("w_out", [D, D])
    w_router = din("w_router", [D, NEXP])
    r_bias = din("router_bias", [NEXP])
    NE_DECL = int(os.environ.get("MK_NEXP", 1 if os.environ.get("MK_STOP", "") else NEXP))
    w_eg = din("w_exp_gate", [NE_DECL, D, FE])
    w_eu = din("w_exp_up", [NE_DECL, D, FE])
    w_ed = din("w_exp_down", [NE_DECL, FE, D])
    w_sg = din("w_sh_gate", [D, FE])
    w_su = din("w_sh_up", [D, FE])
    w_sd = din("w_sh_down", [FE, D])
    ident_in = din("ident", [128, 128])
    tri_in = din("tri", [128, 128])
    ctab_in = din("ctab", [128, 192])
    lt_in = din("ltm", [128, 4096])
    rope_all = din("rope_all", [NKEY, 256])
    rope_own = din("rope_own", [TOK, 256])
    out_d = nc.dram_tensor("out", [TOK, D], F32, kind="ExternalOutput").ap()

    KT_d = nc.dram_tensor("KT_d", [10, 128, NKEY], BF16).ap()
    V_d = nc.dram_tensor("V_d", [NKEY, 1280], BF16).ap()
    SG_d = nc.dram_tensor("SG_d", [32, 128, TOK], BF16).ap()
    gate_d = nc.dram_tensor("gate_d", [4, D], F32).ap()
    Xg_d = nc.dram_tensor("Xg_d", [NSLOT + 1, D], BF16).ap()
    Yg_d = nc.dram_tensor("Yg_d", [NSLOT + 1, D], BF16).ap()
    R_Xg = Res("Xg_d")
    R_Yg = Res("Yg_d")
    R_KT = Res("KT_d")
    R_V = Res("V_d")
    R_SG = Res("SG_d")
    R_gate = Res("gate_d")

    A = Arena(nc, 212800)
    psum = nc.alloc_psum_tensor("psum_all", [128, 8 * 1024], BF16)
    PB = [Res(f"bank{b}") for b in range(8)]

    def bank_f(b):
        return psum[:, b * 1024:(b + 1) * 1024].bitcast(F32)

    def bank_b(b, n=1):
        return psum[:, b * 1024:(b + n) * 1024]

    def mm(out, lhsT, rhs, start, stop, reads, writes):
        S.op('pe', lambda e: e.matmul(out, lhsT=lhsT, rhs=rhs, start=start, stop=stop), reads, writes)

    def tr(out, in_, ident, reads, writes):
        S.op('pe', lambda e: e.transpose(out, in_, ident), reads, writes)

    def actf(out, in_, func, reads, writes, **kw):
        S.op('act', lambda e: e.activation(out=out, in_=in_, func=func, **kw), reads, writes)

    def tt(eng, out, in0, in1, op, reads, writes):
        S.op(eng, lambda e: e.tensor_tensor(out=out, in0=in0, in1=in1, op=op), reads, writes)

    def ts(eng, out, in0, s1, s2, op0, op1, reads, writes):
        if s2 is None:
            S.op(eng, lambda e: e.tensor_scalar(out=out, in0=in0, scalar1=s1, scalar2=None, op0=op0), reads, writes)
        else:
            S.op(eng, lambda e: e.tensor_scalar(out=out, in0=in0, scalar1=s1, scalar2=s2, op0=op0, op1=op1),
                 reads, writes)

    def stt(eng, out, in0, scalar, in1, op0, op1, reads, writes):
        S.op(eng, lambda e: e.scalar_tensor_tensor(out=out, in0=in0, scalar=scalar, in1=in1, op0=op0, op1=op1),
             reads, writes)

    def cp(eng, out, in_, reads, writes):
        if eng == 'act':
            S.op(eng, lambda e: e.activation(out=out, in_=in_, func=AF.Copy), reads, writes)
        else:
            S.op(eng, lambda e: e.tensor_copy(out=out, in_=in_), reads, writes)

    def red(eng, out, in_, op, reads, writes):
        S.op(eng, lambda e: e.tensor_reduce(out=out, in_=in_, axis=AX.X, op=op), reads, writes)

    def mset(eng, ap, val, writes):
        S.op(eng, lambda e: e.memset(ap, val), (), writes)

    def dma(q, out, in_, reads, writes, chan, accw=()):
        S.dma(q, lambda e: e.dma_start(out=out, in_=in_), reads, writes, chan, accw)

    def rstd_ops(eng, out, ss, inv_n, reads, writes):
        ts(eng, out, ss, inv_n, EPS, ALU.mult, ALU.add, reads, writes)
        actf(out, out, AF.Sqrt, writes, writes)
        S.op(eng, lambda e: e.reciprocal(out=out, in_=out), writes, writes)

    STOP = os.environ.get("MK_STOP", "")
    dbg_n = [0]

    def dump(row0, col0, ap, res, dt):
        n = ap.shape[1]
        if dt == F32:
            src = ap
            rr = res
        else:
            tmp, rr = A.alloc([128, n], F32, f"dbg{dbg_n[0]}")
            dbg_n[0] += 1
            cp('dve', tmp, ap, [res], [rr])
            src = tmp
        dma('sp', out_d[row0:row0 + 128, col0:col0 + n], src, [rr], (), 'dbg')

    def finish():
        S.emit()
        return nc

    ident_f, R_idf = A.alloc([128, 128], F32, "ident_f")
    ident_b, R_idb = A.alloc([128, 128], BF16, "ident_b")
    ones_b, R_ones = A.alloc([128, 128], BF16, "ones_b")
    T1, R_T1 = A.alloc([128, 96], F32, "T1")
    T2, R_T2 = A.alloc([128, 64], F32, "T2")
    sT, R_sT = A.alloc([128, 16, 2], BF16, "sT")
    modT, R_modT = A.alloc([128, 96, 2], F32, "modT")
    GS, R_GS = A.alloc([128, 6, 16], F32, "GS")
    gq_a, R_gqa = A.alloc([128, 128], F32, "gq_a")
    gk_a, R_gka = A.alloc([128, 128], F32, "gk_a")
    gq_b, R_gqb = A.alloc([128, 128], F32, "gq_b")
    gk_b, R_gkb = A.alloc([128, 128], F32, "gk_b")
    gsub, R_gsub = A.alloc([128, 256], F32, "gsub")
    rbias, R_rb = A.alloc([128, 64], F32, "rbias")
    neglam, R_nl = A.alloc([128, 1], F32, "neglam")

    dma('sp', ident_f, ident_in, (), [R_idf], 'c0')
    for ap_, src, r_ in ((gq_a, qn_a, R_gqa), (gk_a, kn_a, R_gka), (gq_b, qn_b, R_gqb), (gk_b, kn_b, R_gkb),
                         (gsub, subln, R_gsub), (rbias, r_bias, R_rb)):
        dma('sp', ap_, src.partition_broadcast(128), (), [r_], 'c0')
    mark_const = A.top
    wkv, R_wkv = A.alloc([128, 16, 2560], BF16, "wkv")
    mark_b1 = A.top
    lt, R_lt = A.alloc([128, 4, 128], F32, "lt")
    lp, R_lp = A.alloc([128, 2, 128], F32, "lp")
    ls, R_ls = A.alloc([128, 2], F32, "ls")
    sm1, R_sm1 = A.alloc([96, 128], F32, "sm1")
    sm2, R_sm2 = A.alloc([64, 128], F32, "sm2")
    for i, src in enumerate((lq1, lk1, lq2, lk2)):
        dma('sp', lt[:, i, :], src.partition_broadcast(128), (), [R_lt], 'c0')
    dma('sp', sm1, b_ada.rearrange("(j p) -> j p", p=128), (), [R_sm1], 'c0')
    for i, src in enumerate((norm_mix, norm_ffn, c_in, cctx_in)):
        dma('sp', sm2[i * 16:(i + 1) * 16, :], src.rearrange("(j p) -> j p", p=128), (), [R_sm2], 'c0')
    c0_final = ('d', 'c0', S.chans['c0'][1])
    for r_ in (R_idf, R_gqa, R_gka, R_gqb, R_gkb, R_gsub, R_rb, R_lt, R_sm1, R_sm2):
        r_.w[('d', 'c0')] = c0_final
    cp('dve', ident_b, ident_f, [R_idf], [R_idb])
    mset('dve', ones_b, 1.0, [R_ones])
    ts('dve', gsub, gsub, 1.0 - LAM_INIT, None, ALU.mult, None, [R_gsub], [R_gsub])
    tt('dve', lp[:, 0, :], lt[:, 0, :], lt[:, 1, :], ALU.mult, [R_lt], [R_lp])
    tt('dve', lp[:, 1, :], lt[:, 2, :], lt[:, 3, :], ALU.mult, [R_lt], [R_lp])
    red('dve', ls, lp, ALU.add, [R_lp], [R_ls])
    actf(ls, ls, AF.Exp, [R_ls], [R_ls])
    tt('dve', neglam, ls[:, 1:2], ls[:, 0:1], ALU.subtract, [R_ls], [R_nl])
    ts('dve', neglam, neglam, -LAM_INIT, None, ALU.add, None, [R_nl], [R_nl])
    tr(bank_f(0)[:, 0:96], sm1, ident_f[0:96, 0:96], [R_sm1, R_idf], [PB[0]])
    tr(bank_f(0)[:, 128:192], sm2, ident_f[0:64, 0:64], [R_sm2, R_idf], [PB[0]])
    cp('dve', T1, bank_f(0)[:, 0:96], [PB[0]], [R_T1])
    cp('dve', T2, bank_f(0)[:, 128:192], [PB[0]], [R_T2])
    actf(sT[:, :, 0], T2[:, 32:48], AF.Silu, [R_T2], [R_sT])
    actf(sT[:, :, 1], T2[:, 48:64], AF.Silu, [R_T2], [R_sT])

    wst = []
    for i in range(2):
        wst.append(A.alloc([128, 16, 512], F32, f"wst{i}"))
    w_ada_v = w_ada.rearrange("(kc p) n -> p kc n", p=128)
    psm = bank_f(1)
    for b in range(24):
        wb_, R_wb = wst[b % 2]
        dma('sp', wb_, w_ada_v[:, :, b * 512:(b + 1) * 512], (), [R_wb], f'wst{b % 2}')
        wv = bfv(wb_)
        for jj in range(4):
            j = 4 * b + jj
            for kc in range(16):
                mm(psm[:, 2 * j:2 * j + 2], wv[:, kc, jj * 128:(jj + 1) * 128], sT[:, kc, :],
                   kc == 0, kc == 15, [R_wb, R_sT], [PB[1]])
    tt('dve', modT, psm[:, 0:192].rearrange("p (j r) -> p j r", r=2),
       T1.unsqueeze(2).to_broadcast([128, 96, 2]), ALU.add, [PB[1], R_T1], [R_modT])
    stt('dve', GS[:, 0, :], modT[:, 16:32, 0], 1.0, T2[:, 0:16], ALU.add, ALU.mult, [R_modT, R_T2], [R_GS])
    cp('dve', GS[:, 1, :], modT[:, 0:16, 0], [R_modT], [R_GS])
    stt('dve', GS[:, 2, :], modT[:, 16:32, 1], 1.0, T2[:, 0:16], ALU.add, ALU.mult, [R_modT, R_T2], [R_GS])
    cp('dve', GS[:, 3, :], modT[:, 0:16, 1], [R_modT], [R_GS])
    stt('dve', GS[:, 4, :], modT[:, 64:80, 0], 1.0, T2[:, 16:32], ALU.add, ALU.mult, [R_modT, R_T2], [R_GS])
    cp('dve', GS[:, 5, :], modT[:, 48:64, 0], [R_modT], [R_GS])
    gT, R_gT = A.alloc([128, 4, 16], F32, "gT")
    grow, R_grow = A.alloc([16, 4, 128], F32, "grow")
    cp('dve', gT[:, 0, :], modT[:, 32:48, 0], [R_modT], [R_gT])
    cp('dve', gT[:, 1, :], modT[:, 80:96, 0], [R_modT], [R_gT])
    cp('dve', gT[:, 2:4, :], GS[:, 4:6, :], [R_GS], [R_gT])
    for g in range(4):
        tr(bank_f(0)[0:16, g * 128:(g + 1) * 128], gT[:, g, :], ident_f, [R_gT, R_idf], [PB[0]])
    cp('dve', grow, bank_f(0)[0:16, 0:512].rearrange("p (g n) -> p g n", g=4), [PB[0]], [R_grow])
    for g in range(4):
        dma('sp', gate_d[g].rearrange("(j p) -> j p", p=128), grow[:, g, :], [R_grow], (), 'c_gate', accw=[R_gate])

    w_in_v = w_in.rearrange("(kc p) n -> p kc n", p=128)
    kvcols = (1024, 2560, 3072, 3584, 4096)
    for i, c0 in enumerate(kvcols):
        wb_, R_wb = wst[i % 2]
        dma('sp', wb_, w_in_v[:, :, c0:c0 + 512], (), [R_wb], f'wst{i % 2}')
        cp('dve' if i % 2 == 0 else 'pool', wkv[:, :, i * 512:(i + 1) * 512], wb_, [R_wb], [R_wkv])

    if STOP == "A":
        dump(0, 0, modT.rearrange("p j r -> p (j r)"), R_modT, F32)
        dump(0, 192, GS.rearrange("p a b -> p (a b)"), R_GS, F32)
        dump(0, 288, ls, R_ls, F32)
        dump(0, 320, T2, R_T2, F32)
        return finish()

    def make_norm_bufs(n_in):
        bufs = {}
        bufs['xt'] = [A.alloc([128, D], F32, f"xt{i}") for i in range(n_in)]
        bufs['xs'] = [A.alloc([128, D], BF16, f"xs{i}") for i in range(2)]
        bufs['junk'] = A.alloc([128, D], BF16, "junk")[0]
        bufs['ss'] = [A.alloc([128, 1], F32, f"ss{i}") for i in range(2)]
        bufs['rs'] = [A.alloc([128, 1], F32, f"rs{i}") for i in range(2)]
        return bufs

    def norm_s1(bufs, i, src_ap, src_res, xs_out=None, pre=None):
        b = i % 2
        if src_res is None:
            xt, R_xt = bufs['xt'][b]
            dma('sp', xt, src_ap, (), [R_xt], f'xt{b}')
        else:
            xt, R_xt = src_ap, src_res
        ss, R_ss = bufs['ss'][b]
        rs, R_rs = bufs['rs'][b]
        xs, R_xs = bufs['xs'][b] if xs_out is None else xs_out
        mset('dve', ss, 0.0, [R_ss])
        junk = bufs['junk']
        actf(junk, xt, AF.Square, [R_xt, R_ss], [R_ss], accum_out=ss)
        rstd_ops('dve', rs, ss, 1.0 / D, [R_ss], [R_rs])
        actf(xs, xt, AF.Copy, [R_xt, R_rs], [R_xs], scale=rs[:, 0:1])
        return xs, R_xs

    def norm_s2(xs, R_xs, gi, hT_out, R_hT, pbank0):
        pT = bank_b(pbank0, 2)
        Rp = PB[pbank0]
        for j in range(16):
            tr(pT[:, j * 128:(j + 1) * 128], xs[:, j * 128:(j + 1) * 128], ident_b, [R_xs, R_idb], [Rp, PB[pbank0 + 1]])
        pv = pT.rearrange("p (j t) -> p j t", j=16)
        tt('dve', hT_out, pv, GS[:, gi, :].unsqueeze(2).to_broadcast([128, 16, 128]), ALU.mult,
           [Rp, PB[pbank0 + 1], R_GS], [R_hT])
        tt('dve', hT_out, hT_out, GS[:, gi + 1, :].unsqueeze(2).to_broadcast([128, 16, 128]), ALU.add,
           [R_hT, R_GS], [R_hT])

    def rope_ops(eng, src, R_src, rope_t, R_rope, nh, t1, R_t1, t2, R_t2, out, R_out):
        cos_b = rope_t[:, 0:128].unsqueeze(1).to_broadcast([128, nh, 128])
        tt(eng, t1, src, cos_b, ALU.mult, [R_src, R_rope], [R_t1])
        sv = src.rearrange("p h (a s i) -> p h a s i", a=2, s=2)
        tv = t2.rearrange("p h (a s i) -> p h a s i", a=2, s=2)
        sn = rope_t[:, 128:256].rearrange("p (a s i) -> p a s i", a=2, s=2)
        for s_ in range(2):
            tt(eng, tv[:, :, :, s_, :], sv[:, :, :, 1 - s_, :],
               sn[:, :, s_, :].unsqueeze(1).to_broadcast([128, nh, 2, 32]), ALU.mult, [R_src, R_rope], [R_t2])
        tt(eng, out, t1, t2, ALU.add, [R_t1, R_t2], [R_out])

    S.barrier()
    A.top = mark_b1
    nb = make_norm_bufs(2)
    gainK, R_gK = A.alloc([128, 10, 128], F32, "gainK")
    cp('pool', gainK[:, 0:2, :], gk_a.unsqueeze(1).to_broadcast([128, 2, 128]), [R_gka], [R_gK])
    cp('pool', gainK[:, 2:10, :], gk_b.unsqueeze(1).to_broadcast([128, 8, 128]), [R_gkb], [R_gK])
    hT = [A.alloc([128, 16, 128], BF16, f"hT{i}") for i in range(2)]
    ropet = [A.alloc([128, 256], F32, f"ropet{i}") for i in range(2)]
    vst = [A.alloc([128, 1280], BF16, f"vst{i}") for i in range(2)]
    sqk = [A.alloc([128, 1280], F32, f"sqk{i}") for i in range(2)]
    kss = [A.alloc([128, 10], F32, f"kss{i}") for i in range(2)]
    krs = [A.alloc([128, 10], F32, f"krs{i}") for i in range(2)]
    ksb = [A.alloc([128, 10, 128], F32, f"ksb{i}") for i in range(2)]
    kt1, R_kt1 = A.alloc([128, 10, 128], F32, "kt1")
    kt2, R_kt2 = A.alloc([128, 10, 128], F32, "kt2")
    kro = [A.alloc([128, 10, 128], BF16, f"kro{i}") for i in range(2)]
    kst = [A.alloc([128, 10, 256], BF16, f"kst{i}") for i in range(2)]
    KT_v = KT_d.rearrange("h d s -> d h s")
    xs_of = {}

    def b1_s1(t):
        src = ctx_in[t * 128:(t + 1) * 128, :] if t < 2 else x_all[(t - 2) * 128:(t - 1) * 128, :]
        xs_of[t] = norm_s1(nb, t, src, None)
        rp, R_rp = ropet[t % 2]
        dma('sp', rp, rope_all[t * 128:(t + 1) * 128, :], (), [R_rp], f'rp{t % 2}')

    def b1_s2(t):
        xs, R_xs = xs_of.pop(t)
        h, R_h = hT[t % 2]
        norm_s2(xs, R_xs, 2 if t < 2 else 0, h, R_h, 0)

    def b1_s3(t):
        h, R_h = hT[t % 2]
        for blk in range(5):
            for kc in range(16):
                mm(bank_f(2 + blk), h[:, kc, :], wkv[:, kc, blk * 512:(blk + 1) * 512], kc == 0, kc == 15,
                   [R_h, R_wkv], [PB[2 + blk]])

    def b1_s4(t):
        b = t % 2
        v, R_v = vst[b]
        cp('act', v[:, 0:256], bank_f(2)[:, 256:512], [PB[2]], [R_v])
        cp('act', v[:, 256:768], bank_f(5), [PB[5]], [R_v])
        cp('act', v[:, 768:1280], bank_f(6), [PB[6]], [R_v])
        dma('sp', V_d[t * 128:(t + 1) * 128, :], v, [R_v], (), f'vst{b}', accw=[R_V])
        sq, R_sq = sqk[b]
        actf(sq[:, 0:256], bank_f(2)[:, 0:256], AF.Square, [PB[2]], [R_sq])
        actf(sq[:, 256:768], bank_f(3), AF.Square, [PB[3]], [R_sq])
        actf(sq[:, 768:1280], bank_f(4), AF.Square, [PB[4]], [R_sq])
        ks, R_ks = kss[b]
        kr, R_kr = krs[b]
        red('dve', ks, sq.rearrange("p (h d) -> p h d", h=10), ALU.add, [R_sq], [R_ks])
        rstd_ops('dve', kr, ks, 1.0 / 128, [R_ks], [R_kr])
        kb_, R_kb = ksb[b]
        for (bk, h0, nh, c0) in ((2, 0, 2, 0), (3, 2, 4, 0), (4, 6, 4, 0)):
            tt('dve', kb_[:, h0:h0 + nh, :], bank_f(bk)[:, c0:c0 + nh * 128].rearrange("p (h d) -> p h d", h=nh),
               kr[:, h0:h0 + nh].unsqueeze(2).to_broadcast([128, nh, 128]), ALU.mult, [PB[bk], R_kr], [R_kb])
        tt('pool', kb_, kb_, gainK, ALU.mult, [R_kb, R_gK], [R_kb])
        rp, R_rp = ropet[b]
        ko, R_ko = kro[b]
        rope_ops('pool', kb_, R_kb, rp, R_rp, 10, kt1, R_kt1, kt2, R_kt2, ko, R_ko)

    def b1_s5(t):
        b = t % 2
        ko, R_ko = kro[b]
        gb_ = (t // 2) % 2
        slot = t % 2
        kq, R_kq = kst[gb_]
        for g in range(2):
            for h in range(5):
                tr(bank_b(7)[:, h * 128:(h + 1) * 128], ko[:, 5 * g + h, :], ident_b, [R_ko, R_idb], [PB[7]])
            cp('act', kq[:, 5 * g:5 * g + 5, slot * 128:(slot + 1) * 128],
               bank_b(7)[:, 0:640].rearrange("p (h t) -> p h t", h=5), [PB[7]], [R_kq])
        if slot == 1:
            dma('sp', KT_v[:, :, (t - 1) * 128:(t + 1) * 128], kq, [R_kq], (), f'kst{gb_}', accw=[R_KT])

    NT_RUN = int(os.environ.get('MK_NT', NST))
    for i in range(-2, NT_RUN + 1):
        if 0 <= i + 2 < NT_RUN:
            b1_s1(i + 2)
        if 0 <= i + 1 < NT_RUN:
            b1_s2(i + 1)
        if 0 <= i < NT_RUN:
            b1_s3(i)
            b1_s4(i)
        if 0 <= i - 1 < NT_RUN:
            b1_s5(i - 1)

    if STOP == "B1":
        S.barrier()
        A.top = mark_const
        nk = 2048 if NT_RUN == NST else 128 * NT_RUN
        for i_, (hh, s0) in enumerate(((0, 0), (2, 0), (9, 6400 if NT_RUN == NST else 0))):
            db, R_db = A.alloc([128, nk], BF16, f"db{i_}")
            dma('sp', db, KT_d[hh][:, s0:s0 + nk], [R_KT], [R_db], f'dbl{i_}')
            dump(128 * i_, 0, db, R_db, BF16)
        for i_, r0 in enumerate((0, 8320 if NT_RUN == NST else 128)):
            db, R_db = A.alloc([128, 1280], BF16, f"dv{i_}")
            dma('sp', db, V_d[r0:r0 + 128, :], [R_V], [R_db], f'dvl{i_}')
            dump(384 + 128 * i_, 0, db, R_db, BF16)
        return finish()

    S.barrier()
    A.top = mark_const
    topoff = A.n2 - 2 * 8 * TOK
    oaT = A.base[:, topoff:topoff + 8 * TOK].rearrange("p (a b) -> p a b", a=8)
    obT = A.base[:, topoff + 8 * TOK:topoff + 16 * TOK].rearrange("p (a b) -> p a b", a=8)
    R_oaT = Res("oaT")
    R_obT = Res("obT")
    qT, R_qT = A.alloc([128, 16, TOK], BF16, "qT")
    mark_c = A.top
    hTo, R_hTo = A.alloc([128, 16, TOK], BF16, "hT_own")
    nb = make_norm_bufs(2)
    ropeo, R_ropeo = A.alloc([128, NTT, 256], F32, "ropeo")
    dma('sp', ropeo, rope_own.rearrange("(t p) c -> p t c", p=128), (), [R_ropeo], 'c_ropeo')
    wq = [A.alloc([128, 16, 512], F32, f"wq{i}") for i in range(2)]
    sqq, R_sqq = A.alloc([128, 512], F32, "sqq")
    qss = [A.alloc([128, 4], F32, f"qss{i}") for i in range(2)]
    qrs = [A.alloc([128, 4], F32, f"qrs{i}") for i in range(2)]
    qsb = [A.alloc([128, 4, 128], F32, f"qsb{i}") for i in range(2)]
    qt1, R_qt1 = A.alloc([128, 4, 128], F32, "qt1")
    qt2, R_qt2 = A.alloc([128, 4, 128], F32, "qt2")
    qro = [A.alloc([128, 4, 128], BF16, f"qro{i}") for i in range(2)]
    sgst = [A.alloc([128, 512], BF16, f"sgst{i}") for i in range(2)]

    for t_ in range(NTT + 1):
        if t_ < NTT:
            xs_of[t_] = norm_s1(nb, t_, x_own[t_ * 128:(t_ + 1) * 128, :], None)
        if t_ >= 1:
            xs, R_xs = xs_of.pop(t_ - 1)
            norm_s2(xs, R_xs, 0, hTo[:, :, (t_ - 1) * 128:t_ * 128], R_hTo, 0)

    wqi = 0
    for qb in range(4):
        c0 = qb * 512 if qb < 2 else 1536 + (qb - 2) * 512
        gq, R_gq = (gq_a, R_gqa) if qb < 2 else (gq_b, R_gqb)
        w_, R_w = wq[wqi % 2]
        dma('sp', w_, w_in_v[:, :, c0:c0 + 512], (), [R_w], f'wq{wqi % 2}')
        wqi += 1
        wv = bfv(w_)

        def q_front(t_):
            pb = 2 + t_ % 2
            i2 = t_ % 2
            for kc in range(16):
                mm(bank_f(pb), hTo[:, kc, t_ * 128:(t_ + 1) * 128], wv[:, kc, :], kc == 0, kc == 15,
                   [R_hTo, R_w], [PB[pb]])
            actf(sqq, bank_f(pb), AF.Square, [PB[pb]], [R_sqq])
            red('dve', qss[i2][0], sqq.rearrange("p (h d) -> p h d", h=4), ALU.add, [R_sqq], [qss[i2][1]])
            rstd_ops('dve', qrs[i2][0], qss[i2][0], 1.0 / 128, [qss[i2][1]], [qrs[i2][1]])
            qs_, R_qs = qsb[i2]
            tt('dve', qs_, bank_f(pb).rearrange("p (h d) -> p h d", h=4),
               qrs[i2][0].unsqueeze(2).to_broadcast([128, 4, 128]), ALU.mult, [PB[pb], qrs[i2][1]], [R_qs])
            tt('pool', qs_, qs_, gq.unsqueeze(1).to_broadcast([128, 4, 128]), ALU.mult, [R_qs, R_gq], [R_qs])
            rope_ops('pool', qs_, R_qs, ropeo[:, t_, :], R_ropeo, 4, qt1, R_qt1, qt2, R_qt2, qro[i2][0], qro[i2][1])

        def q_back(t_):
            i2 = t_ % 2
            for h in range(4):
                tr(bank_b(4)[:, h * 128:(h + 1) * 128], qro[i2][0][:, h, :], ident_b, [qro[i2][1], R_idb], [PB[4]])
            cp('act', qT[:, 4 * qb:4 * qb + 4, t_ * 128:(t_ + 1) * 128],
               bank_b(4)[:, 0:512].rearrange("p (h t) -> p h t", h=4), [PB[4]], [R_qT])

        for t_ in range(NTT + 1):
            if t_ < NTT:
                q_front(t_)
            if t_ >= 1:
                q_back(t_ - 1)

    cnt = 0
    for gbk in range(8):
        c0 = 4608 + gbk * 512
        w_, R_w = wq[wqi % 2]
        dma('sp', w_, w_in_v[:, :, c0:c0 + 512], (), [R_w], f'wq{wqi % 2}')
        wqi += 1
        wv = bfv(w_)
        for ch in range(4):
            for half in range(2):
                pb = 5 + cnt % 2
                for kc in range(16):
                    mm(bank_f(pb), wv[:, kc, ch * 128:(ch + 1) * 128], hTo[:, kc, half * 512:(half + 1) * 512],
                       kc == 0, kc == 15, [R_hTo, R_w], [PB[pb]])
                sg_, R_sg = sgst[cnt % 2]
                actf(sg_, bank_f(pb), AF.Sigmoid, [PB[pb]], [R_sg])
                dma('sp', SG_d[4 * gbk + ch][:, half * 512:(half + 1) * 512], sg_, [R_sg], (), f'sg{cnt % 2}',
                    accw=[R_SG])
                cnt += 1

    S.barrier()
    A.top = mark_c
    A.limit = topoff
    zt, R_zt = A.alloc([128, D], BF16, "zt")
    mset('pool', zt, 0.0, [R_zt])
    for r0 in range(0, NSLOT, 128):
        nr_ = min(128, NSLOT - r0)
        dma('pool', Xg_d[r0:r0 + nr_, :], zt[0:nr_, :], [R_zt], (), 'zfill', accw=[R_Xg])
        if int(os.environ.get('MK_NRANK', NEXP)) < NEXP:
            dma('pool', Yg_d[r0:r0 + nr_, :], zt[0:nr_, :], [R_zt], (), 'zfill', accw=[R_Yg])
    dma('pool', Xg_d[NSLOT:NSLOT + 1, :], zt[0:1, :], [R_zt], (), 'zfill', accw=[R_Xg])
    dma('pool', Yg_d[NSLOT:NSLOT + 1, :], zt[0:1, :], [R_zt], (), 'zfill', accw=[R_Yg])
    ostg_g, R_og = A.alloc([128, 4, 128], BF16, "ostg_g")
    ostg_d, R_od = A.alloc([128, 4, 256], BF16, "ostg_d")
    kbuf = [A.alloc([128, NKEY], BF16, f"kbuf{i}") for i in range(2)]
    vraw = [A.alloc([128, NST * 257], BF16, f"vraw{i}") for i in range(2)]
    ebuf = [A.alloc([128, 512], BF16, f"ebuf{i}") for i in range(3)]
    obf = [A.alloc([128, 4, 256], F32, f"obf{i}") for i in range(2)]
    osq, R_osq = A.alloc([128, 4, 256], F32, "osq")
    rec, R_rec = A.alloc([128, 4], F32, "rec")
    rec2, R_rec2 = A.alloc([128, 4], F32, "rec2")
    ssb, R_ssb = A.alloc([128, 4], F32, "ssb")
    rsb, R_rsb = A.alloc([128, 4], F32, "rsb")
    V_v = V_d.rearrange("(t p) c -> p t c", p=128)
    kcnt = [0]
    vcnt = [0]

    NSA = NT_RUN

    def load_k(idx):
        i = kcnt[0] % 2
        kcnt[0] += 1
        kb_, R_kb = kbuf[i]
        dma('sp', kb_[:, 0:NSA * 128], KT_d[idx][:, 0:NSA * 128], [R_KT], [R_kb], f'kb{i}')
        return kb_, R_kb

    def load_v(c0, dv):
        i = vcnt[0] % 2
        vcnt[0] += 1
        raw, R_raw = vraw[i]
        view = raw[:, 0:NST * (dv + 1)].rearrange("p (t c) -> p t c", c=dv + 1)
        mset('pool', view[:, 0:NSA, dv:dv + 1], 1.0, [R_raw])
        dma('sp', view[:, 0:NSA, 0:dv], V_v[:, 0:NSA, c0:c0 + dv], [R_V], [R_raw], f'vb{i}')
        return view, R_raw

    po_banks = (3, 4, 5, 6)

    def attn_block(kb_, R_kb, vv, R_v, dv, qh, qb):
        def Sm(st):
            mm(bank_f(st % 3), kb_[:, st * 128:(st + 1) * 128], qT[:, qh, qb * 512:(qb + 1) * 512], True, True,
               [R_kb, R_qT], [PB[st % 3]])

        def Ex(st):
            e_, R_e = ebuf[st % 3]
            actf(e_, bank_f(st % 3), AF.Exp, [PB[st % 3]], [R_e], scale=SCALE)

        def PV(st):
            e_, R_e = ebuf[st % 3]
            for q4 in range(4):
                mm(bank_f(po_banks[q4])[:, 0:dv + 1], e_[:, q4 * 128:(q4 + 1) * 128], vv[:, st, :],
                   st == 0, st == NSA - 1, [R_e, R_v], [PB[po_banks[q4]]])
        Sm(0)
        Sm(1)
        for st in range(NSA):
            Ex(st)
            if st + 2 < NSA:
                Sm(st + 2)
            PV(st)

    jobs = [('g', 0), ('g', 1), ('d', 0), ('d', 1), ('d', 2), ('d', 3)]

    def job_v(job):
        kind, i = job
        if kind == 'g':
            return load_v(i * 128, 128)
        return load_v(256 + i * 256, 256)

    def job_k(job):
        kind, i = job
        if kind == 'g':
            return [load_k(i)]
        return [load_k(2 + 2 * i), load_k(2 + 2 * i + 1)]

    loaded_v = job_v(jobs[0])
    for ji, job in enumerate(jobs):
        cur_v = loaded_v
        ks_ = job_k(job)
        if ji + 1 < len(jobs):
            loaded_v = job_v(jobs[ji + 1])
        cur = (ks_, cur_v)
        kind, i = job
        ks_, (vv, R_v) = cur
        if kind == 'g':
            kb_, R_kb = ks_[0]
            for qh in range(4 * i, 4 * i + 4):
                for qb in range(2):
                    attn_block(kb_, R_kb, vv, R_v, 128, qh, qb)
                    for q4 in range(4):
                        pb = po_banks[q4]
                        S.op('dve', (lambda o_, i_: lambda e: e.reciprocal(out=o_, in_=i_))(rec[:, q4:q4 + 1], bank_f(pb)[:, 128:129]),
                             [PB[pb]], [R_rec])
                        ts('dve', ostg_g[:, q4, :], bank_f(pb)[:, 0:128],
                           rec[:, q4:q4 + 1], None, ALU.mult, None, [PB[pb], R_rec], [R_og])
                    for q4 in range(4):
                        tr(bank_b(7)[:, q4 * 128:(q4 + 1) * 128], ostg_g[:, q4, :], ident_b, [R_og, R_idb], [PB[7]])
                    cp('dve', oaT[:, qh, qb * 512:(qb + 1) * 512], bank_b(7)[:, 0:512], [PB[7]], [R_oaT])
        else:
            for qb in range(2):
                of_, R_of = obf[qb % 2]
                for m in range(2):
                    kb_, R_kb = ks_[m]
                    attn_block(kb_, R_kb, vv, R_v, 256, 8 + 2 * i + m, qb)
                    for q4 in range(4):
                        pb = po_banks[q4]
                        if m == 0:
                            S.op('dve', (lambda o_, i_: lambda e: e.reciprocal(out=o_, in_=i_))(rec[:, q4:q4 + 1], bank_f(pb)[:, 256:257]),
                                 [PB[pb]], [R_rec])
                            ts('dve', of_[:, q4, :], bank_f(pb)[:, 0:256], rec[:, q4:q4 + 1], None, ALU.mult, None,
                               [PB[pb], R_rec], [R_of])
                        else:
                            S.op('dve', (lambda o_, i_: lambda e: e.reciprocal(out=o_, in_=i_))(rec2[:, q4:q4 + 1], bank_f(pb)[:, 256:257]),
                                 [PB[pb]], [R_rec2])
                            ts('dve', rec2[:, q4:q4 + 1], rec2[:, q4:q4 + 1], neglam[:, 0:1], None, ALU.mult, None,
                               [R_rec2, R_nl], [R_rec2])
                            stt('dve', of_[:, q4, :], bank_f(pb)[:, 0:256], rec2[:, q4:q4 + 1], of_[:, q4, :],
                                ALU.mult, ALU.add, [PB[pb], R_rec2, R_of], [R_of])
                tt('dve', osq, of_, of_, ALU.mult, [R_of], [R_osq])
                red('dve', ssb, osq, ALU.add, [R_osq], [R_ssb])
                rstd_ops('dve', rsb, ssb, 1.0 / 256, [R_ssb], [R_rsb])
                tt('dve', of_, of_, rsb.unsqueeze(2).to_broadcast([128, 4, 256]), ALU.mult, [R_of, R_rsb], [R_of])
                tt('dve', ostg_d, of_,
                   gsub.unsqueeze(1).to_broadcast([128, 4, 256]), ALU.mult, [R_of, R_gsub], [R_od])
                for c_ in range(2):
                    for q4 in range(4):
                        tr(bank_b(7)[:, (c_ * 4 + q4) * 128:(c_ * 4 + q4 + 1) * 128],
                           ostg_d[:, q4, c_ * 128:(c_ + 1) * 128], ident_b, [R_od, R_idb], [PB[7]])
                cp('dve', obT[:, 2 * i:2 * i + 2, qb * 512:(qb + 1) * 512],
                   bank_b(7).rearrange("p (c t) -> p c t", c=2), [PB[7]], [R_obT])

    if STOP == "C":
        S.barrier()
        return finish()

    S.barrier()
    A.top = mark_const
    yT, R_yT = A.alloc([128, 16, TOK], BF16, "yT")
    mark_d2 = A.top
    wab = [A.alloc([128, 8, 256], F32, f"wab{i}") for i in range(4)]
    sgl = [A.alloc([128, 2, TOK], BF16, f"sgl{i}") for i in range(2)]
    ytm = [A.alloc([128, 512], F32, f"ytm{i}") for i in range(4)]
    wa_v = w_ba.rearrange("(ac p) n -> p ac n", p=128)
    wb_v = w_bb.rearrange("(ac p) n -> p ac n", p=128)
    k = 0
    for cb in range(8):
        wa_, R_wa = wab[cb % 2]
        wb2, R_wb2 = wab[2 + cb % 2]
        dma('sp', wa_, wa_v[:, :, cb * 256:(cb + 1) * 256], (), [R_wa], f'wab{cb % 2}')
        dma('sp', wb2, wb_v[:, :, cb * 256:(cb + 1) * 256], (), [R_wb2], f'wab{2 + cb % 2}')
        wav = bfv(wa_)
        wbv = bfv(wb2)
        for dcl in range(2):
            dc = 2 * cb + dcl
            sg_, R_sg = sgl[dc % 2]
            dma('sp', sg_[:, 0, :], SG_d[dc], [R_SG], [R_sg], f'sgl{dc % 2}')
            dma('sp', sg_[:, 1, :], SG_d[16 + dc], [R_SG], [R_sg], f'sgl{dc % 2}')
            for half in range(2):
                pa = k % 2
                pbk = 2 + k % 2
                hs = slice(half * 512, (half + 1) * 512)
                for ac in range(8):
                    mm(bank_f(pa), wav[:, ac, dcl * 128:(dcl + 1) * 128], oaT[:, ac, hs], ac == 0, ac == 7,
                       [R_wa, R_oaT], [PB[pa]])
                for ac in range(8):
                    mm(bank_f(pbk), wbv[:, ac, dcl * 128:(dcl + 1) * 128], obT[:, ac, hs], ac == 0, ac == 7,
                       [R_wb2, R_obT], [PB[pbk]])
                ya, R_ya = ytm[(2 * k) % 4]
                yb, R_yb = ytm[(2 * k + 1) % 4]
                tt('dve', ya, bank_f(pa), sg_[:, 0, hs], ALU.mult, [PB[pa], R_sg], [R_ya])
                tt('dve', yb, bank_f(pbk), sg_[:, 1, hs], ALU.mult, [PB[pbk], R_sg], [R_yb])
                tt('pool', yT[:, dc, hs], ya, yb, ALU.add, [R_ya, R_yb], [R_yT])
                k += 1

    S.barrier()
    A.top = mark_d2
    A.limit = A.n2
    x1, R_x1 = A.alloc([128, NTT, D], F32, "x1")
    dma('sp', x1, x_own.rearrange("(t p) d -> p t d", p=128), (), [R_x1], 'x1ld')
    wo = [A.alloc([128, 16, 256], F32, f"wo{i}") for i in range(2)]
    gateB, R_gB = A.alloc([128, D], F32, "gateB")
    otm = [A.alloc([128, 512], F32, f"otm{i}") for i in range(2)]
    dma('sp', gateB, gate_d[0].partition_broadcast(128), [R_gate], [R_gB], 'c_gB')
    wo_v = w_out.rearrange("(dc p) n -> p dc n", p=128)
    k = 0
    for cb in range(8):
        w_, R_w = wo[cb % 2]
        dma('sp', w_, wo_v[:, :, cb * 256:(cb + 1) * 256], (), [R_w], f'wo{cb % 2}')
        wv = bfv(w_)
        cs = slice(cb * 256, (cb + 1) * 256)
        for t_ in range(NTT):
            pb = k % 2
            for dc in range(16):
                mm(bank_f(pb)[:, 0:256], yT[:, dc, t_ * 128:(t_ + 1) * 128], wv[:, dc, :], dc == 0, dc == 15,
                   [R_yT, R_w], [PB[pb]])
            o_, R_o = otm[k % 2]
            o_ = o_[:, 0:256]
            tt('dve', o_, bank_f(pb)[:, 0:256], gateB[:, cs], ALU.mult, [PB[pb], R_gB], [R_o])
            tt('pool', x1[:, t_, cs], x1[:, t_, cs], o_, ALU.add, [R_x1, R_o], [R_x1])
            k += 1

    X1_d = nc.dram_tensor("X1_d", [TOK, D], F32).ap()
    R_X1d = Res("X1_d")
    dma('sp', X1_d.rearrange("(t p) d -> p t d", p=128), x1, [R_x1], [R_X1d], 'x1st')
    S.barrier()
    A.top = mark_const
    X2_d = nc.dram_tensor("X2_d", [TOK, D], F32).ap()
    R_X2d = Res("X2_d")
    slw, R_slw = A.alloc([128, NTT, 2, 8], F32, "slw")
    slots_i, R_sli = A.alloc([128, NTT, 8], I32, "slots_i")
    idxw, R_idxw = A.alloc([128, 2, 64], I32, "idxw")
    mark_keep = A.top
    h2T, R_h2T = A.alloc([128, 16, TOK], BF16, "h2T")
    mark_h2 = A.top
    xs2, R_xs2 = A.alloc([128, NTT, D], BF16, "xs2")
    wn, R_wn = A.alloc([128, NTT, 64], F32, "wn")
    selb, R_selb = A.alloc([128, NTT, 64], BF16, "selb")
    bmk_all, R_bmka = A.alloc([128, NTT, 64], F32, "bmk_all")
    t8_all, R_t8a = A.alloc([128, NTT, 8], F32, "t8_all")
    tri_f, R_trif = A.alloc([128, 128], F32, "tri_f")
    tri_b, R_trib = A.alloc([128, 128], BF16, "tri_b")
    dma('sp', tri_f, tri_in, (), [R_trif], 'c_tri')
    cp('dve', tri_b, tri_f, [R_trif], [R_trib])
    nb = make_norm_bufs(2)
    wr, R_wr = A.alloc([128, 16, NEXP], F32, "wr")
    dma('sp', wr, w_router.rearrange("(kc p) e -> p kc e", p=128), (), [R_wr], 'c_wr')
    wrv = bfv(wr)
    rt = {}
    for nm, shp in (('sc', [128, 512]), ('bi', [128, 512]), ('m1', [128, 64]), ('eq', [128, 512]), ('bm', [128, 512]),
                    ('m2', [128, 64]), ('gs', [128, 64]), ('cmp', [128, 512]), ('rank', [128, 64]), ('gm', [128, 64]),
                    ('thr', [128, 8]), ('sel', [128, 512]), ('ws', [128, 512]), ('wsum', [128, 8]),
                    ('sf', [128, 512]), ('oh', [128, 512]), ('pr', [128, 512])):
        rt[nm] = A.alloc(shp, F32, "rt_" + nm)
    for t_ in range(NTT + 1):
        if t_ < NTT:
            xs_of[t_] = norm_s1(nb, t_, X1_d[t_ * 128:(t_ + 1) * 128, :], None, xs_out=(xs2[:, t_, :], R_xs2))
        if t_ >= 1:
            xs, R_xs = xs_of.pop(t_ - 1)
            norm_s2(xs, R_xs, 4, h2T[:, :, (t_ - 1) * 128:t_ * 128], R_h2T, 0)

    GSB, R_GSB = A.alloc([128, 2, D], F32, "GSB")
    dma('sp', GSB[:, 0, :], gate_d[2].partition_broadcast(128), [R_gate], [R_GSB], 'c_gsb')
    dma('sp', GSB[:, 1, :], gate_d[3].partition_broadcast(128), [R_gate], [R_GSB], 'c_gsb')
    for t_ in range(NTT):
        tt('pool', xs2[:, t_, :], xs2[:, t_, :], GSB[:, 0, :], ALU.mult, [R_xs2, R_GSB], [R_xs2])
        tt('pool', xs2[:, t_, :], xs2[:, t_, :], GSB[:, 1, :], ALU.add, [R_xs2, R_GSB], [R_xs2])

    def v3(nm):
        return rt[nm][0].rearrange("p (t e) -> p t e", t=NTT)

    def v4(nm):
        return rt[nm][0].rearrange("p (q i) -> p q i", i=8)

    def vq(nm):
        return rt[nm][0].rearrange("p (t g) -> p t g", t=NTT)
    for t_ in range(NTT):
        for kc in range(16):
            mm(bank_f(2)[:, t_ * 64:(t_ + 1) * 64], h2T[:, kc, t_ * 128:(t_ + 1) * 128], wrv[:, kc, :],
               kc == 0, kc == 15, [R_h2T, R_wr], [PB[2]])
    sc, R_sc = rt['sc']
    bi, R_bi = rt['bi']
    actf(sc, bank_f(2), AF.Sigmoid, [PB[2]], [R_sc])
    tt('dve', v3('bi'), v3('sc'), rbias.unsqueeze(1).to_broadcast([128, NTT, 64]), ALU.add, [R_sc, R_rb], [R_bi])
    S.op('dve', lambda e: e.tensor_reduce(out=rt['m1'][0], in_=v4('bi'), axis=AX.X, op=ALU.max), [R_bi], [rt['m1'][1]])
    tt('dve', v4('eq'), v4('bi'), rt['m1'][0].unsqueeze(2).to_broadcast([128, 64, 8]), ALU.is_equal,
       [R_bi, rt['m1'][1]], [rt['eq'][1]])
    stt('dve', rt['bm'][0], rt['eq'][0], -1e9, bi, ALU.mult, ALU.add, [rt['eq'][1], R_bi], [rt['bm'][1]])
    S.op('dve', lambda e: e.tensor_reduce(out=rt['m2'][0], in_=v4('bm'), axis=AX.X, op=ALU.max), [rt['bm'][1]], [rt['m2'][1]])
    tt('dve', rt['gs'][0], rt['m1'][0], rt['m2'][0], ALU.add, [rt['m1'][1], rt['m2'][1]], [rt['gs'][1]])
    gq = vq('gs')
    cmp4 = rt['cmp'][0].rearrange("p (t g h) -> p t g h", t=NTT, g=8)
    tt('dve', cmp4, gq.unsqueeze(2).to_broadcast([128, NTT, 8, 8]), gq.unsqueeze(3).to_broadcast([128, NTT, 8, 8]),
       ALU.is_gt, [rt['gs'][1]], [rt['cmp'][1]])
    S.op('dve', lambda e: e.tensor_reduce(out=rt['rank'][0], in_=v4('cmp'), axis=AX.X, op=ALU.add), [rt['cmp'][1]], [rt['rank'][1]])
    ts('dve', rt['gm'][0], rt['rank'][0], 3.5, None, ALU.is_lt, None, [rt['rank'][1]], [rt['gm'][1]])
    ts('dve', rt['gm'][0], rt['gm'][0], -1.0, 1e9, ALU.add, ALU.mult, [rt['gm'][1]], [rt['gm'][1]])
    bmk4 = bmk_all.rearrange("p t (g i) -> p (t g) i", i=8)
    tt('dve', bmk4, v4('bi'), rt['gm'][0].unsqueeze(2).to_broadcast([128, 64, 8]), ALU.add,
       [R_bi, rt['gm'][1]], [R_bmka])
    for t_ in range(NTT):
        S.op('dve', (lambda o_, i_: lambda e: e.max(out=o_, in_=i_))(t8_all[:, t_, :], bmk_all[:, t_, :]),
             [R_bmka], [R_t8a])
    S.op('dve', lambda e: e.tensor_reduce(out=rt['thr'][0], in_=t8_all, axis=AX.X, op=ALU.min), [R_t8a], [rt['thr'][1]])
    tt('dve', v3('sel'), bmk_all, rt['thr'][0].unsqueeze(2).to_broadcast([128, NTT, 64]), ALU.is_ge,
       [R_bmka, rt['thr'][1]], [rt['sel'][1]])
    cp('dve', selb, v3('sel'), [rt['sel'][1]], [R_selb])
    tt('dve', rt['ws'][0], sc, rt['sel'][0], ALU.mult, [R_sc, rt['sel'][1]], [rt['ws'][1]])
    red('dve', rt['wsum'][0], v3('ws'), ALU.add, [rt['ws'][1]], [rt['wsum'][1]])
    S.op('dve', lambda e: e.reciprocal(out=rt['wsum'][0], in_=rt['wsum'][0]), [rt['wsum'][1]], [rt['wsum'][1]])
    stt('dve', wn, v3('ws'), 2.5, rt['wsum'][0].unsqueeze(2).to_broadcast([128, NTT, 64]), ALU.mult, ALU.mult,
        [rt['ws'][1], rt['wsum'][1]], [R_wn])

    cnt, R_cnt = A.alloc([128, 64], F32, "cnt")
    rank, R_rank = A.alloc([128, 64], F32, "rank")
    ebase, R_ebase = A.alloc([128, 64], F32, "ebase")
    pif, R_pif = A.alloc([128, 64], F32, "pif")
    c3a, R_c3a = A.alloc([128, 16, 64], F32, "c3a")
    c3b, R_c3b = A.alloc([128, 16, 64], F32, "c3b")
    ltc, R_ltc = A.alloc([128, 16, 64], F32, "ltc")
    ctab, R_ctab = A.alloc([128, 3, 64], F32, "ctab")
    dma('sp', ctab, ctab_in.rearrange("p (a b) -> p a b", a=3), (), [R_ctab], 'c_ctab')
    iota64 = ctab[:, 0, :]
    btab = ctab[:, 1, :]
    for t2 in range(NTT):
        mm(bank_f(6)[:, 0:64], ones_b, selb[:, t2, :], t2 == 0, t2 == NTT - 1, [R_ones, R_selb], [PB[6]])
    cp('dve', cnt, bank_f(6)[:, 0:64], [PB[6]], [R_cnt])
    cnt_e2 = cnt.unsqueeze(1).to_broadcast([128, 16, 64])
    for ch in range(4):
        cs_ = slice(ch * 16, ch * 16 + 16)
        dma('sp', ltc, lt_in[:, ch * 1024:(ch + 1) * 1024].rearrange("p (a b) -> p a b", a=16), (), [R_ltc], 'c_ltc')
        cnt_e = cnt[:, cs_].unsqueeze(2).to_broadcast([128, 16, 64])
        tt('dve', c3a, cnt_e2, cnt_e, ALU.is_gt, [R_cnt], [R_c3a])
        tt('dve', c3b, cnt_e2, cnt_e, ALU.is_equal, [R_cnt], [R_c3b])
        tt('dve', c3b, c3b, ltc, ALU.mult, [R_c3b, R_ltc], [R_c3b])
        tt('dve', c3a, c3a, c3b, ALU.add, [R_c3a, R_c3b], [R_c3a])
        red('dve', rank[:, cs_], c3a, ALU.add, [R_c3a], [R_rank])
    for ch in range(4):
        cs_ = slice(ch * 16, ch * 16 + 16)
        tt('dve', c3a, rank[:, cs_].unsqueeze(2).to_broadcast([128, 16, 64]),
           iota64.unsqueeze(1).to_broadcast([128, 16, 64]), ALU.is_equal, [R_rank, R_ctab], [R_c3a])
        tt('dve', c3a, c3a, btab.unsqueeze(1).to_broadcast([128, 16, 64]), ALU.mult, [R_c3a, R_ctab], [R_c3a])
        red('dve', ebase[:, cs_], c3a, ALU.add, [R_c3a], [R_ebase])
        tt('dve', c3b, rank.unsqueeze(1).to_broadcast([128, 16, 64]),
           iota64[:, cs_].unsqueeze(2).to_broadcast([128, 16, 64]), ALU.is_equal, [R_rank, R_ctab], [R_c3b])
        tt('dve', c3b, c3b, iota64.unsqueeze(1).to_broadcast([128, 16, 64]), ALU.mult, [R_c3b, R_ctab], [R_c3b])
        red('dve', pif[:, cs_], c3b, ALU.add, [R_c3b], [R_pif])
    ts('dve', pif, pif, 128.0, ctab[:, 2, 0:1], ALU.mult, ALU.add, [R_pif, R_ctab], [R_pif])
    ts('dve', pif, pif, 2.0, None, ALU.mult, None, [R_pif], [R_pif])
    cp('dve', idxw[:, 0, :], pif, [R_pif], [R_idxw])
    ts('dve', pif, pif, 1.0, None, ALU.add, None, [R_pif], [R_pif])
    cp('dve', idxw[:, 1, :], pif, [R_pif], [R_idxw])

    for t_ in range(NTT):
        pp = bank_f(4)[:, t_ * 64:(t_ + 1) * 64]
        for t2 in range(t_ + 1):
            mm(pp, ones_b if t2 < t_ else tri_b, selb[:, t2, :], t2 == 0, t2 == t_,
               [R_ones, R_trib, R_selb], [PB[4]])
    sf3 = v3('sf')
    tt('dve', sf3, bank_f(4).rearrange("p (t e) -> p t e", t=NTT), ebase.unsqueeze(1).to_broadcast([128, NTT, 64]),
       ALU.add, [PB[4], R_ebase], [rt['sf'][1]])
    ts('dve', rt['sf'][0], rt['sf'][0], -float(TRASH), None, ALU.add, None, [rt['sf'][1]], [rt['sf'][1]])
    tt('dve', rt['sf'][0], rt['sf'][0], rt['sel'][0], ALU.mult, [rt['sf'][1], rt['sel'][1]], [rt['sf'][1]])
    ts('dve', rt['sf'][0], rt['sf'][0], float(TRASH), None, ALU.add, None, [rt['sf'][1]], [rt['sf'][1]])
    for k_ in range(8):
        tt('dve', v3('oh'), bmk_all, t8_all[:, :, k_:k_ + 1].to_broadcast([128, NTT, 64]), ALU.is_equal,
           [R_bmka, R_t8a], [rt['oh'][1]])
        tt('dve', rt['pr'][0], rt['oh'][0], rt['sf'][0], ALU.mult, [rt['oh'][1], rt['sf'][1]], [rt['pr'][1]])
        red('dve', slw[:, :, 0, k_], v3('pr'), ALU.add, [rt['pr'][1]], [R_slw])
        tt('dve', v3('pr'), v3('oh'), wn, ALU.mult, [rt['oh'][1], R_wn], [rt['pr'][1]])
        red('dve', slw[:, :, 1, k_], v3('pr'), ALU.add, [rt['pr'][1]], [R_slw])
    cp('dve', slots_i, slw[:, :, 0, :], [R_slw], [R_sli])

    def scatter(t_, k_):
        off = bass.IndirectOffsetOnAxis(ap=slots_i[:, t_, k_:k_ + 1], axis=0)
        src = xs2[:, t_, :]
        S.dma('pool', lambda e: e.indirect_dma_start(out=Xg_d[:, :], out_offset=off, in_=src, in_offset=None),
              [R_xs2, R_sli], (), 'scat', accw=[R_Xg])
    for t_ in range(NTT):
        for k_ in range(8):
            scatter(t_, k_)

    S.barrier()
    A.top = mark_h2
    x1, R_x1 = A.alloc([128, NTT, D], F32, "acc")
    ring = [A.alloc([128, 4096], F32, f"ring{i}") for i in range(4)]
    actTd, R_ad = A.alloc([128, 4, TOK], BF16, "actTd")
    sil = [A.alloc([128, 512], F32, f"sil{i}") for i in range(2)]
    gateB2, R_gB2 = A.alloc([128, D], F32, "gateB2")
    dma('sp', gateB2, gate_d[1].partition_broadcast(128), [R_gate], [R_gB2], 'c_gB2')
    rcnt = [0]

    def ring_load(src_ap, shape3):
        i = rcnt[0] % 4
        rcnt[0] += 1
        r_, R_r = ring[i]
        view = r_.rearrange("p (a b) -> p a b", a=shape3[0])
        dma('sp', view, src_ap, (), [R_r], f'ring{i}')
        return view, R_r

    def shared_expert(wg_ap, wu_ap, wd_ap):
        a_, R_a = actTd, R_ad
        wgv = wg_ap.rearrange("(kc p) f -> p kc f", p=128)
        wuv = wu_ap.rearrange("(kc p) f -> p kc f", p=128)
        wdv = wd_ap.rearrange("(fc p) d -> p fc d", p=128)
        kk = 0
        for pair in range(2):
            g_, R_g = ring_load(wgv[:, :, pair * 256:(pair + 1) * 256], [16, 256])
            u_, R_u = ring_load(wuv[:, :, pair * 256:(pair + 1) * 256], [16, 256])
            gv = bfv(g_)
            uv = bfv(u_)
            for fcl in range(2):
                fc = 2 * pair + fcl
                for half in range(2):
                    pg = (2 * kk) % 4
                    pu = (2 * kk + 1) % 4
                    hs = slice(half * 512, (half + 1) * 512)
                    for kc in range(16):
                        mm(bank_f(pg), gv[:, kc, fcl * 128:(fcl + 1) * 128], h2T[:, kc, hs], kc == 0, kc == 15,
                           [R_g, R_h2T], [PB[pg]])
                    for kc in range(16):
                        mm(bank_f(pu), uv[:, kc, fcl * 128:(fcl + 1) * 128], h2T[:, kc, hs], kc == 0, kc == 15,
                           [R_u, R_h2T], [PB[pu]])
                    s_, R_s = sil[kk % 2]
                    actf(s_, bank_f(pg), AF.Silu, [PB[pg]], [R_s])
                    tt('dve', a_[:, fc, hs], bank_f(pu), s_, ALU.mult, [PB[pu], R_s], [R_a])
                    kk += 1
        d0, R_d0 = ring_load(wdv[:, 0:2, :], [2, 2048])
        d1, R_d1 = ring_load(wdv[:, 2:4, :], [2, 2048])
        dv0 = bfv(d0)
        dv1 = bfv(d1)
        kk = 0
        for t_ in range(NTT):
            for cb in range(4):
                pb = 4 + kk % 4
                cs = slice(cb * 512, (cb + 1) * 512)
                for fc in range(4):
                    dvv, R_dd = (dv0, R_d0) if fc < 2 else (dv1, R_d1)
                    mm(bank_f(pb), a_[:, fc, t_ * 128:(t_ + 1) * 128], dvv[:, fc % 2, cs], fc == 0, fc == 3,
                       [R_a, R_dd], [PB[pb]])
                tt('dve', x1[:, t_, cs], bank_f(pb), gateB2[:, cs], ALU.mult, [PB[pb], R_gB2], [R_x1])
                kk += 1
            xr_, R_xr = xrs[0]
            dma('sp', xr_, X1_d[t_ * 128:(t_ + 1) * 128, :], [R_X1d], [R_xr], 'xrs0')
            tt('pool', xr_, xr_, x1[:, t_, :], ALU.add, [R_xr, R_x1], [R_xr])
            dma('sp', X2_d[t_ * 128:(t_ + 1) * 128, :], xr_, [R_xr], (), 'xrs0', accw=[R_X2d])

    xrs = [A.alloc([128, D], F32, f"xrs{i}") for i in range(1)]
    shared_expert(w_sg, w_su, w_sd)

    S.barrier()
    A.top = mark_keep
    NRING = 8
    ringb = [A.alloc([128, 4096], F32, f"ringb{i}") for i in range(NRING)]
    XeT, R_XeT = A.alloc([128, 16, 1024], BF16, "XeT")
    xe = [A.alloc([128, D], BF16, f"xe{i}") for i in range(2)]
    actSt = [A.alloc([128, 4, 128], BF16, f"actSt{i}") for i in range(2)]
    aTok, R_aTok = A.alloc([128, 8, 512], BF16, "aTok")
    ysb = [A.alloc([128, D], BF16, f"ysb{i}") for i in range(2)]
    GS2, R_GS2 = A.alloc([128, 2, 16], F32, "GS2")
    dma('sp', GS2, gate_d[2:4, :].rearrange("g (p k) -> p g k", k=16), [R_gate], [R_GS2], 'c_gs2')
    Wg_rows = w_eg.rearrange("e (p h k) f -> (e p h) (k f)", h=2, k=8)
    Wu_rows = w_eu.rearrange("e (p h k) f -> (e p h) (k f)", h=2, k=8)
    Wd_rows = w_ed.rearrange("e (p h c) d -> (e p h) (c d)", h=2, c=2)
    NRANK = int(os.environ.get('MK_NRANK', NEXP))
    cnts = {'xe': 0, 'y': 0, 'k': 0, 'w': 0}

    def wgather(rows_ap, half, r):
        i = cnts['w'] % NRING
        cnts['w'] += 1
        p_, R_p = ringb[i]
        off = bass.IndirectOffsetOnAxis(ap=idxw[:, half, r:r + 1], axis=0)
        src = rows_ap[:, :]
        S.dma('pool', lambda e: e.indirect_dma_start(out=p_, out_offset=None, in_=src, in_offset=off),
              [R_idxw], [R_p], f'ringb{i}')
        return p_, R_p

    WTS = {}

    def build_xet(r):
        C = CAPS[r]
        base = BASES[r]
        tiles = [(s0, min(128, C - s0)) for s0 in range(0, C, 128)]
        glo = wgather(Wg_rows, 0, r)
        ghi = wgather(Wg_rows, 1, r)
        ulo = wgather(Wu_rows, 0, r)
        uhi = wgather(Wu_rows, 1, r)
        dlo = wgather(Wd_rows, 0, r)
        dhi = wgather(Wd_rows, 1, r)
        WTS[r] = (glo, ghi, ulo, uhi, dlo, dhi)
        for (s0, ns) in tiles:
            xb, R_xb = xe[cnts['xe'] % 2]
            pbk = 2 * (cnts['xe'] % 2)
            cnts['xe'] += 1
            dma('sp', xb[0:ns, :], Xg_d[base + s0:base + s0 + ns, :], [R_Xg], [R_xb], f'xe{(cnts["xe"] - 1) % 2}')
            pX = bank_b(pbk, 2)
            Rp = [PB[pbk], PB[pbk + 1]]
            xv = xb[0:ns, :].rearrange("s (p k) -> s k p", k=16)
            for kc in range(16):
                tr(pX[:, kc * 128:kc * 128 + ns], xv[:, kc, :], ident_b[0:ns, 0:ns], [R_xb, R_idb], Rp)
            pv = pX.rearrange("p (k s) -> p k s", s=128)[:, :, 0:ns]
            cp('act' if cnts['xe'] % 2 == 0 else 'dve', XeT[:, :, s0:s0 + ns], pv, Rp, [R_XeT])


    def passes12(r):
        C = CAPS[r]
        base = BASES[r]
        tiles = [(s0, min(128, C - s0)) for s0 in range(0, C, 128)]
        glo, ghi, ulo, uhi, dlo, dhi = WTS[r]
        def wmov(lo, hi, kc):
            p_ = lo[0] if kc < 8 else hi[0]
            R_ = lo[1] if kc < 8 else hi[1]
            v = p_.bitcast(BF16).rearrange("p (k f two) -> p k f two", k=8, two=2)[:, kc % 8, :, 1]
            return v, R_
        for ti, (s0, ns) in enumerate(tiles):
            k_ = cnts['k']
            cnts['k'] += 1
            pbk = 4 + k_ % 2
            pg = bank_f(pbk)[0:ns, :]
            for kc in range(16):
                v, R_ = wmov(glo, ghi, kc)
                mm(pg, XeT[:, kc, s0:s0 + ns], v, kc == 0, kc == 15, [R_, R_XeT], [PB[pbk]])
            actf(aTok[0:ns, ti, :], pg, AF.Silu, [PB[pbk]], [R_aTok])
        for ti, (s0, ns) in enumerate(tiles):
            k_ = cnts['k']
            cnts['k'] += 1
            pbk = 4 + k_ % 2
            pu = bank_f(pbk)[0:ns, :]
            for kc in range(16):
                v, R_ = wmov(ulo, uhi, kc)
                mm(pu, XeT[:, kc, s0:s0 + ns], v, kc == 0, kc == 15, [R_, R_XeT], [PB[pbk]])
            tt('dve', aTok[0:ns, ti, :], pu, aTok[0:ns, ti, :], ALU.mult, [PB[pbk], R_aTok], [R_aTok])

    def passes34(r):
        C = CAPS[r]
        base = BASES[r]
        tiles = [(s0, min(128, C - s0)) for s0 in range(0, C, 128)]
        glo, ghi, ulo, uhi, dlo, dhi = WTS[r]
        for ti, (s0, ns) in enumerate(tiles):
            k_ = cnts['y']
            cnts['y'] += 1
            pbk = k_ % 2
            pT_ = bank_b(pbk)
            as_, R_as = actSt[k_ % 2]
            av = aTok[0:ns, ti, :].rearrange("s (m c) -> s c m", c=4)
            for fc in range(4):
                tr(pT_[:, fc * 128:fc * 128 + ns], av[:, fc, :], ident_b[0:ns, 0:ns], [R_aTok, R_idb], [PB[pbk]])
            cp('dve', as_[:, :, 0:ns], pT_[:, 0:512].rearrange("p (c s) -> p c s", c=4)[:, :, 0:ns], [PB[pbk]], [R_as])
            yi = k_ % 2
            y_, R_y = ysb[yi]
            for cb in range(4):
                pb = 6 + cb % 2
                cs = slice(cb * 512, (cb + 1) * 512)
                for fc in range(4):
                    p_, R_ = (dlo if fc < 2 else dhi)
                    dvv = p_.bitcast(BF16).rearrange("p (c d two) -> p c d two", c=2, two=2)[:, fc % 2, :, 1]
                    mm(bank_f(pb)[0:ns, :], as_[:, fc, 0:ns], dvv[:, cs], fc == 0, fc == 3, [R_as, R_], [PB[pb]])
                cp('act', y_[0:ns, cs], bank_f(pb)[0:ns, :], [PB[pb]], [R_y])
            dma('sp', Yg_d[base + s0:base + s0 + ns, :], y_[0:ns, :], [R_y], (), f'ysb{yi}', accw=[R_Yg])

    order = []
    lo_, hi_ = 0, NRANK - 1
    while lo_ <= hi_:
        order.append(lo_)
        if hi_ != lo_:
            order.append(hi_)
        lo_ += 1
        hi_ -= 1
    build_xet(order[0])
    for i_, r in enumerate(order):
        passes12(r)
        if i_ + 1 < len(order):
            build_xet(order[i_ + 1])
        passes34(r)

    S.barrier()
    A.top = mark_keep
    accs = [A.alloc([128, D], F32, f"accs{i}") for i in range(2)]
    ytm = [A.alloc([128, D], BF16, f"ytm{i}") for i in range(4)]
    gateB3, R_gB3 = A.alloc([128, D], F32, "gateB3")
    xr = [A.alloc([128, D], F32, f"xr{i}") for i in range(2)]
    dma('sp', gateB3, gate_d[1].partition_broadcast(128), [R_gate], [R_gB3], 'c_gB3')
    gcnt = [0]
    for t_ in range(NTT):
        ac_, R_ac = accs[t_ % 2]
        for k_ in range(8):
            i = gcnt[0] % 4
            gcnt[0] += 1
            y_, R_y = ytm[i]
            off = bass.IndirectOffsetOnAxis(ap=slots_i[:, t_, k_:k_ + 1], axis=0)
            S.dma('pool', (lambda y__, off_: lambda e: e.indirect_dma_start(out=y__, out_offset=None, in_=Yg_d[:, :],
                                                                            in_offset=off_))(y_, off),
                  [R_Yg, R_sli], [R_y], f'ytm{i}')
            if k_ == 0:
                ts('dve', ac_, y_, slw[:, t_, 1, 0:1], None, ALU.mult, None, [R_y, R_slw], [R_ac])
            else:
                stt('dve', ac_, y_, slw[:, t_, 1, k_:k_ + 1], ac_, ALU.mult, ALU.add, [R_y, R_slw, R_ac], [R_ac])
        xr_, R_xr = xr[t_ % 2]
        dma('sp', xr_, X2_d[t_ * 128:(t_ + 1) * 128, :], [R_X2d], [R_xr], f'xr{t_ % 2}')
        tt('dve', ac_, ac_, gateB3, ALU.mult, [R_ac, R_gB3], [R_ac])
        tt('pool', xr_, xr_, ac_, ALU.add, [R_xr, R_ac], [R_xr])
        dma('sp', out_d[t_ * 128:(t_ + 1) * 128, :], xr_, [R_xr], (), f'xr{t_ % 2}')

    S.emit()
    return nc


_CACHE = {}


def _ctab():
    c = np.zeros((128, 3, 64), np.float32)
    c[:, 0, :] = np.arange(64, dtype=np.float32)[None, :]
    c[:, 1, :] = np.asarray(BASES, np.float32)[None, :]
    c[:, 2, 0] = np.arange(128, dtype=np.float32)
    return np.ascontiguousarray(c.reshape(128, 192))


def _ltm():
    m = (np.arange(64)[None, :] < np.arange(64)[:, None]).astype(np.float32)
    return np.ascontiguousarray(np.tile(m.reshape(1, 4096), (128, 1)))


def _rope_tables():
    rows_n = SEQ // 64
    rows = np.repeat(np.arange(rows_n, dtype=np.float32), 64)
    cols = np.tile(np.arange(64, dtype=np.float32), rows_n)
    inv = (np.float32(10000.0) ** (-np.arange(32, dtype=np.float32) / np.float32(32))).astype(np.float32)
    ang_r = (rows[:, None] * inv).astype(np.float32)
    ang_c = (cols[:, None] * inv).astype(np.float32)
    cr, sr, cc, sc = np.cos(ang_r), np.sin(ang_r), np.cos(ang_c), np.sin(ang_c)
    cos4 = np.concatenate([cr, cr, cc, cc], axis=1)
    sin4 = np.concatenate([-sr, sr, -sc, sc], axis=1)
    lat = np.concatenate([cos4, sin4], axis=1).astype(np.float32)
    ctxp = np.concatenate([np.ones((CTX, 128), np.float32), np.zeros((CTX, 128), np.float32)], axis=1)
    return np.ascontiguousarray(np.concatenate([ctxp, lat], axis=0)), np.ascontiguousarray(lat)


def kernel(**inputs):
    NE_RUN = int(os.environ.get('MK_NEXP', 1 if os.environ.get('MK_STOP', '') else NEXP))
    f = lambda k: np.ascontiguousarray(np.asarray(inputs[k], dtype=np.float32))
    if 'nc' not in _CACHE:
        _CACHE['nc'] = build_program()
    nc = _CACHE['nc']
    rope_all, rope_lat = _rope_tables()
    x = f('x')[0]
    shared = {
        'x_all': x, 'ctx': f('ctx')[0], 'c': f('c')[0], 'c_ctx': f('c_ctx'),
        'w_ada': f('w_ada')[0], 'b_ada': f('b_ada')[0], 'norm_mix': f('norm_mix')[0], 'norm_ffn': f('norm_ffn')[0],
        'w_in': f('w_in')[0], 'q_norm_a': f('q_norm_a')[0], 'k_norm_a': f('k_norm_a')[0],
        'q_norm_b': f('q_norm_b')[0], 'k_norm_b': f('k_norm_b')[0],
        'lambda_q1': f('lambda_q1')[0], 'lambda_k1': f('lambda_k1')[0], 'lambda_q2': f('lambda_q2')[0],
        'lambda_k2': f('lambda_k2')[0], 'subln_b': f('subln_b')[0],
        'w_branch_a': f('w_branch_a')[0], 'w_branch_b': f('w_branch_b')[0], 'w_out': f('w_out')[0],
        'w_router': f('w_router')[0], 'router_bias': f('router_bias')[0],
        'w_exp_gate': f('w_exp_gate')[0][:NE_RUN], 'w_exp_up': f('w_exp_up')[0][:NE_RUN], 'w_exp_down': f('w_exp_down')[0][:NE_RUN],
        'w_sh_gate': f('w_sh_gate')[0], 'w_sh_up': f('w_sh_up')[0], 'w_sh_down': f('w_sh_down')[0],
        'ident': np.eye(128, dtype=np.float32), 'rope_all': rope_all,
        'tri': np.triu(np.ones((128, 128), np.float32), 1),
        'ctab': _ctab(), 'ltm': _ltm(),
    }
    in_maps = []
    for c in range(NCORE):
        m = dict(shared)
        m['x_own'] = np.ascontiguousarray(x[c * TOK:(c + 1) * TOK])
        m['rope_own'] = np.ascontiguousarray(rope_lat[c * TOK:(c + 1) * TOK])
        in_maps.append(m)
    res = run_bass_kernel_spmd(nc, in_maps, core_ids=list(range(NCORE)))
    out = np.concatenate([np.asarray(r['out'], dtype=np.float32) for r in res.results], axis=0)
    return out.reshape(1, SEQ, D)
```

```python
import os
import numpy as np
import concourse.bass as bass
import concourse.mybir as mybir
from concourse.bass_utils import run_bass_kernel_spmd

F32 = mybir.dt.float32
BF16 = mybir.dt.bfloat16
I32 = mybir.dt.int32
ALU = mybir.AluOpType
AF = mybir.ActivationFunctionType
AX = mybir.AxisListType

D = 2048
NCORE = 8
TOK = 1024
NTT = TOK // 128
SEQ = 8192
CTX = 256
NKEY = SEQ + CTX
NST = NKEY // 128
EPS = 1e-6
INW = 8704
NEXP = 64
FE = 512
SCALE = 128 ** -0.5
CAPS = [((min(1024, 8192 // (r + 1)) + 63) // 64) * 64 for r in range(NEXP)]
BASES = [sum(CAPS[:r]) for r in range(NEXP)]
TRASH = sum(CAPS)
NSLOT = TRASH
LAM_INIT = 0.2
ENG = ('pe', 'act', 'dve', 'pool', 'sp')
SEM_LIMIT = 8000


class Res:
    __slots__ = ('name', 'w', 'r')

    def __init__(self, name):
        self.name = name
        self.w = {}
        self.r = {}


class Op:
    pass


class Sched:
    def __init__(self, nc):
        self.nc = nc
        self.ops = {e: [] for e in ENG}
        self.waited = {e: {} for e in ENG}
        self.chans = {}

    def _need(self, eng, toks):
        waits = []
        for tok in toks:
            if tok[0] == 'e':
                _, peng, op = tok
                if peng == 'pe' and eng == 'pe':
                    continue
                key = ('e', peng)
                if self.waited[eng].get(key, -1) >= op.idx:
                    continue
                self.waited[eng][key] = op.idx
                op.marked = True
                waits.append(tok)
            else:
                _, ch, val = tok
                key = ('d', ch)
                if self.waited[eng].get(key, 0) >= val:
                    continue
                self.waited[eng][key] = val
                waits.append(tok)
        return waits

    def _deps(self, reads, writes):
        toks = []
        for r in reads:
            toks += list(r.w.values())
        for w in writes:
            toks += list(w.w.values()) + list(w.r.values())
        return toks

    def op(self, eng, fn, reads=(), writes=()):
        o = Op()
        o.eng = eng
        o.fn = fn
        o.idx = len(self.ops[eng])
        o.marked = False
        o.dma = None
        o.waits = self._need(eng, self._deps(reads, writes))
        self.ops[eng].append(o)
        tok = ('e', eng, o)
        for r in reads:
            r.r[('e', eng)] = tok
        for w in writes:
            w.w[('e', eng)] = tok
            w.r = {}
        return o

    def dma(self, q, fn, reads=(), writes=(), chan=None, accw=()):
        ch = self.chans.setdefault(chan, [None, 0])
        ch[1] += 16
        val = ch[1]
        o = Op()
        o.eng = q
        o.fn = fn
        o.idx = len(self.ops[q])
        o.marked = False
        o.dma = (chan, val)
        toks = self._deps(reads, writes)
        for w in accw:
            toks += list(w.r.values())
        o.waits = self._need(q, toks)
        self.ops[q].append(o)
        tok = ('d', chan, val)
        for r in reads:
            r.r[('d', chan)] = tok
        for w in writes:
            w.w[('d', chan)] = tok
            w.r = {}
        for w in accw:
            w.w[('d', chan)] = tok
        return o

    def barrier(self):
        last = {}
        for e in ENG:
            for o in reversed(self.ops[e]):
                if o.dma is None and o.fn is not None:
                    last[e] = o
                    break
        for e in ENG:
            toks = [('e', pe, o) for pe, o in last.items() if not (pe == e and e in ('pe', 'sp'))]
            toks += [('d', ch, v[1]) for ch, v in self.chans.items()]
            w = self._need(e, toks)
            if w:
                o = Op()
                o.eng = e
                o.fn = None
                o.idx = len(self.ops[e])
                o.marked = False
                o.dma = None
                o.waits = w
                self.ops[e].append(o)

    def emit(self):
        nc = self.nc
        nsem = 0
        for e in ENG:
            cnt = 0
            sem = None
            for o in self.ops[e]:
                if o.marked:
                    if sem is None or cnt >= SEM_LIMIT:
                        sem = nc.alloc_semaphore(f"pg_{e}_{nsem}")
                        nsem += 1
                        cnt = 0
                    cnt += 1
                    o.sem = sem
                    o.val = cnt
        for i, (ch, v) in enumerate(self.chans.items()):
            v[0] = nc.alloc_semaphore(f"ch_{i}")
        sched = self

        def run(engname):
            def body(e):
                for o in sched.ops[engname]:
                    for tok in o.waits:
                        if tok[0] == 'e':
                            e.wait_ge(tok[2].sem, tok[2].val)
                        else:
                            e.wait_ge(sched.chans[tok[1]][0], tok[2])
                    if o.fn is None:
                        continue
                    ins = o.fn(e)
                    if o.dma is not None:
                        ins.then_inc(sched.chans[o.dma[0]][0], 16)
                    elif o.marked:
                        ins.then_inc(o.sem, 1)
                if engname == 'sp':
                    for ch, v in sched.chans.items():
                        if v[1] > 0:
                            e.wait_ge(v[0], v[1])
            return body

        with nc.Block() as block:
            block.tensor(run('pe'))
            block.scalar(run('act'))
            block.vector(run('dve'))
            block.gpsimd(run('pool'))
            block.sync(run('sp'))


DBG_OFFS = {}


class Arena:
    def __init__(self, nc, nbytes):
        self.n2 = nbytes // 2
        self.base = nc.alloc_sbuf_tensor("arena", [128, self.n2], BF16)
        self.top = 0
        self.limit = self.n2

    def alloc(self, shape, dtype, name):
        P = shape[0]
        n = 1
        for s in shape[1:]:
            n *= s
        esz = 4 if dtype in (F32, I32) else 2
        n2 = (n * esz + 1) // 2
        n2 = (n2 + 15) // 16 * 16
        off = self.top
        self.top += n2
        DBG_OFFS[name] = (off, list(shape), str(dtype))
        assert self.top <= self.limit, f"SBUF arena overflow at {name}: {self.top * 2} > {self.limit * 2}"
        ap = self.base[0:P, off:off + (n * esz) // 2]
        if dtype != BF16:
            ap = ap.bitcast(dtype)
        if len(shape) == 3:
            ap = ap.rearrange("p (a b) -> p a b", a=shape[1])
        elif len(shape) == 4:
            ap = ap.rearrange("p (a b c) -> p a b c", a=shape[1], b=shape[2])
        return ap, Res(name)


def bfv(ap):
    v = ap.bitcast(BF16)
    if len(ap.shape) == 2:
        return v.rearrange("p (n two) -> p n two", two=2)[:, :, 1]
    return v.rearrange("p k (n two) -> p k n two", two=2)[:, :, :, 1]


def build_program():
    nc = bass.Bass("TRN2", target_bir_lowering=False)
    S = Sched(nc)

    def din(name, shape, dt=F32):
        return nc.dram_tensor(name, shape, dt, kind="ExternalInput").ap()

    x_all = din("x_all", [SEQ, D])
    x_own = din("x_own", [TOK, D])
    ctx_in = din("ctx", [CTX, D])
    c_in = din("c", [D])
    cctx_in = din("c_ctx", [D])
    w_ada = din("w_ada", [D, 6 * D])
    b_ada = din("b_ada", [6 * D])
    norm_mix = din("norm_mix", [D])
    norm_ffn = din("norm_ffn", [D])
    w_in = din("w_in", [D, INW])
    qn_a = din("q_norm_a", [128])
    kn_a = din("k_norm_a", [128])
    qn_b = din("q_norm_b", [128])
    kn_b = din("k_norm_b", [128])
    lq1 = din("lambda_q1", [128])
    lk1 = din("lambda_k1", [128])
    lq2 = din("lambda_q2", [128])
    lk2 = din("lambda_k2", [128])
    subln = din("subln_b", [256])
    w_ba = din("w_branch_a", [1024, D])
    w_bb = din("w_branch_b", [1024, D])
    w_out = din("w_out", [D, D])
    w_router = din("w_router", [D, NEXP])
    r_bias = din("router_bias", [NEXP])
    NE_DECL = int(os.environ.get("MK_NEXP", 1 if os.environ.get("MK_STOP", "") else NEXP))
    w_eg = din("w_exp_gate", [NE_DECL, D, FE])
    w_eu = din("w_exp_up", [NE_DECL, D, FE])
    w_ed = din("w_exp_down", [NE_DECL, FE, D])
    w_sg = din("w_sh_gate", [D, FE])
    w_su = din("w_sh_up", [D, FE])
    w_sd = din("w_sh_down", [FE, D])
    ident_in = din("ident", [128, 128])
    tri_in = din("tri", [128, 128])
    ctab_in = din("ctab", [128, 192])
    lt_in = din("ltm", [128, 4096])
    rope_all = din("rope_all", [NKEY, 256])
    rope_own = din("rope_own", [TOK, 256])
    out_d = nc.dram_tensor("out", [TOK, D], F32, kind="ExternalOutput").ap()

    KT_d = nc.dram_tensor("KT_d", [10, 128, NKEY], BF16).ap()
    V_d = nc.dram_tensor("V_d", [NKEY, 1280], BF16).ap()
    SG_d = nc.dram_tensor("SG_d", [32, 128, TOK], BF16).ap()
    gate_d = nc.dram_tensor("gate_d", [4, D], F32).ap()
    Xg_d = nc.dram_tensor("Xg_d", [NSLOT + 1, D], BF16).ap()
    Yg_d = nc.dram_tensor("Yg_d", [NSLOT + 1, D], BF16).ap()
    R_Xg = Res("Xg_d")
    R_Yg = Res("Yg_d")
    R_KT = Res("KT_d")
    R_V = Res("V_d")
    R_SG = Res("SG_d")
    R_gate = Res("gate_d")

    A = Arena(nc, 212800)
    psum = nc.alloc_psum_tensor("psum_all", [128, 8 * 1024], BF16)
    PB = [Res(f"bank{b}") for b in range(8)]

    def bank_f(b):
        return psum[:, b * 1024:(b + 1) * 1024].bitcast(F32)

    def bank_b(b, n=1):
        return psum[:, b * 1024:(b + n) * 1024]

    def mm(out, lhsT, rhs, start, stop, reads, writes):
        S.op('pe', lambda e: e.matmul(out, lhsT=lhsT, rhs=rhs, start=start, stop=stop), reads, writes)

    def tr(out, in_, ident, reads, writes):
        S.op('pe', lambda e: e.transpose(out, in_, ident), reads, writes)

    def actf(out, in_, func, reads, writes, **kw):
        S.op('act', lambda e: e.activation(out=out, in_=in_, func=func, **kw), reads, writes)

    def tt(eng, out, in0, in1, op, reads, writes):
        S.op(eng, lambda e: e.tensor_tensor(out=out, in0=in0, in1=in1, op=op), reads, writes)

    def ts(eng, out, in0, s1, s2, op0, op1, reads, writes):
        if s2 is None:
            S.op(eng, lambda e: e.tensor_scalar(out=out, in0=in0, scalar1=s1, scalar2=None, op0=op0), reads, writes)
        else:
            S.op(eng, lambda e: e.tensor_scalar(out=out, in0=in0, scalar1=s1, scalar2=s2, op0=op0, op1=op1),
                 reads, writes)

    def stt(eng, out, in0, scalar, in1, op0, op1, reads, writes):
        S.op(eng, lambda e: e.scalar_tensor_tensor(out=out, in0=in0, scalar=scalar, in1=in1, op0=op0, op1=op1),
             reads, writes)

    def cp(eng, out, in_, reads, writes):
        if eng == 'act':
            S.op(eng, lambda e: e.activation(out=out, in_=in_, func=AF.Copy), reads, writes)
        else:
            S.op(eng, lambda e: e.tensor_copy(out=out, in_=in_), reads, writes)

    def red(eng, out, in_, op, reads, writes):
        S.op(eng, lambda e: e.tensor_reduce(out=out, in_=in_, axis=AX.X, op=op), reads, writes)

    def mset(eng, ap, val, writes):
        S.op(eng, lambda e: e.memset(ap, val), (), writes)

    def dma(q, out, in_, reads, writes, chan, accw=()):
        S.dma(q, lambda e: e.dma_start(out=out, in_=in_), reads, writes, chan, accw)

    def rstd_ops(eng, out, ss, inv_n, reads, writes):
        ts(eng, out, ss, inv_n, EPS, ALU.mult, ALU.add, reads, writes)
        actf(out, out, AF.Sqrt, writes, writes)
        S.op(eng, lambda e: e.reciprocal(out=out, in_=out), writes, writes)

    STOP = os.environ.get("MK_STOP", "")
    dbg_n = [0]

    def dump(row0, col0, ap, res, dt):
        n = ap.shape[1]
        if dt == F32:
            src = ap
            rr = res
        else:
            tmp, rr = A.alloc([128, n], F32, f"dbg{dbg_n[0]}")
            dbg_n[0] += 1
            cp('dve', tmp, ap, [res], [rr])
            src = tmp
        dma('sp', out_d[row0:row0 + 128, col0:col0 + n], src, [rr], (), 'dbg')

    def finish():
        S.emit()
        return nc

    ident_f, R_idf = A.alloc([128, 128], F32, "ident_f")
    ident_b, R_idb = A.alloc([128, 128], BF16, "ident_b")
    ones_b, R_ones = A.alloc([128, 128], BF16, "ones_b")
    T1, R_T1 = A.alloc([128, 96], F32, "T1")
    T2, R_T2 = A.alloc([128, 64], F32, "T2")
    sT, R_sT = A.alloc([128, 16, 2], BF16, "sT")
    modT, R_modT = A.alloc([128, 96, 2], F32, "modT")
    GS, R_GS = A.alloc([128, 6, 16], F32, "GS")
    gq_a, R_gqa = A.alloc([128, 128], F32, "gq_a")
    gk_a, R_gka = A.alloc([128, 128], F32, "gk_a")
    gq_b, R_gqb = A.alloc([128, 128], F32, "gq_b")
    gk_b, R_gkb = A.alloc([128, 128], F32, "gk_b")
    gsub, R_gsub = A.alloc([128, 256], F32, "gsub")
    rbias, R_rb = A.alloc([128, 64], F32, "rbias")
    neglam, R_nl = A.alloc([128, 1], F32, "neglam")

    dma('sp', ident_f, ident_in, (), [R_idf], 'c0')
    for ap_, src, r_ in ((gq_a, qn_a, R_gqa), (gk_a, kn_a, R_gka), (gq_b, qn_b, R_gqb), (gk_b, kn_b, R_gkb),
                         (gsub, subln, R_gsub), (rbias, r_bias, R_rb)):
        dma('sp', ap_, src.partition_broadcast(128), (), [r_], 'c0')
    mark_const = A.top
    wkv, R_wkv = A.alloc([128, 16, 2560], BF16, "wkv")
    mark_b1 = A.top
    lt, R_lt = A.alloc([128, 4, 128], F32, "lt")
    lp, R_lp = A.alloc([128, 2, 128], F32, "lp")
    ls, R_ls = A.alloc([128, 2], F32, "ls")
    sm1, R_sm1 = A.alloc([96, 128], F32, "sm1")
    sm2, R_sm2 = A.alloc([64, 128], F32, "sm2")
    for i, src in enumerate((lq1, lk1, lq2, lk2)):
        dma('sp', lt[:, i, :], src.partition_broadcast(128), (), [R_lt], 'c0')
    dma('sp', sm1, b_ada.rearrange("(j p) -> j p", p=128), (), [R_sm1], 'c0')
    for i, src in enumerate((norm_mix, norm_ffn, c_in, cctx_in)):
        dma('sp', sm2[i * 16:(i + 1) * 16, :], src.rearrange("(j p) -> j p", p=128), (), [R_sm2], 'c0')
    c0_final = ('d', 'c0', S.chans['c0'][1])
    for r_ in (R_idf, R_gqa, R_gka, R_gqb, R_gkb, R_gsub, R_rb, R_lt, R_sm1, R_sm2):
        r_.w[('d', 'c0')] = c0_final
    cp('dve', ident_b, ident_f, [R_idf], [R_idb])
    mset('dve', ones_b, 1.0, [R_ones])
    ts('dve', gsub, gsub, 1.0 - LAM_INIT, None, ALU.mult, None, [R_gsub], [R_gsub])
    tt('dve', lp[:, 0, :], lt[:, 0, :], lt[:, 1, :], ALU.mult, [R_lt], [R_lp])
    tt('dve', lp[:, 1, :], lt[:, 2, :], lt[:, 3, :], ALU.mult, [R_lt], [R_lp])
    red('dve', ls, lp, ALU.add, [R_lp], [R_ls])
    actf(ls, ls, AF.Exp, [R_ls], [R_ls])
    tt('dve', neglam, ls[:, 1:2], ls[:, 0:1], ALU.subtract, [R_ls], [R_nl])
    ts('dve', neglam, neglam, -LAM_INIT, None, ALU.add, None, [R_nl], [R_nl])
    tr(bank_f(0)[:, 0:96], sm1, ident_f[0:96, 0:96], [R_sm1, R_idf], [PB[0]])
    tr(bank_f(0)[:, 128:192], sm2, ident_f[0:64, 0:64], [R_sm2, R_idf], [PB[0]])
    cp('dve', T1, bank_f(0)[:, 0:96], [PB[0]], [R_T1])
    cp('dve', T2, bank_f(0)[:, 128:192], [PB[0]], [R_T2])
    actf(sT[:, :, 0], T2[:, 32:48], AF.Silu, [R_T2], [R_sT])
    actf(sT[:, :, 1], T2[:, 48:64], AF.Silu, [R_T2], [R_sT])

    wst = []
    for i in range(2):
        wst.append(A.alloc([128, 16, 512], F32, f"wst{i}"))
    w_ada_v = w_ada.rearrange("(kc p) n -> p kc n", p=128)
    psm = bank_f(1)
    for b in range(24):
        wb_, R_wb = wst[b % 2]
        dma('sp', wb_, w_ada_v[:, :, b * 512:(b + 1) * 512], (), [R_wb], f'wst{b % 2}')
        wv = bfv(wb_)
        for jj in range(4):
            j = 4 * b + jj
            for kc in range(16):
                mm(psm[:, 2 * j:2 * j + 2], wv[:, kc, jj * 128:(jj + 1) * 128], sT[:, kc, :],
                   kc == 0, kc == 15, [R_wb, R_sT], [PB[1]])
    tt('dve', modT, psm[:, 0:192].rearrange("p (j r) -> p j r", r=2),
       T1.unsqueeze(2).to_broadcast([128, 96, 2]), ALU.add, [PB[1], R_T1], [R_modT])
    stt('dve', GS[:, 0, :], modT[:, 16:32, 0], 1.0, T2[:, 0:16], ALU.add, ALU.mult, [R_modT, R_T2], [R_GS])
    cp('dve', GS[:, 1, :], modT[:, 0:16, 0], [R_modT], [R_GS])
    stt('dve', GS[:, 2, :], modT[:, 16:32, 1], 1.0, T2[:, 0:16], ALU.add, ALU.mult, [R_modT, R_T2], [R_GS])
    cp('dve', GS[:, 3, :], modT[:, 0:16, 1], [R_modT], [R_GS])
    stt('dve', GS[:, 4, :], modT[:, 64:80, 0], 1.0, T2[:, 16:32], ALU.add, ALU.mult, [R_modT, R_T2], [R_GS])
    cp('dve', GS[:, 5, :], modT[:, 48:64, 0], [R_modT], [R_GS])
    gT, R_gT = A.alloc([128, 4, 16], F32, "gT")
    grow, R_grow = A.alloc([16, 4, 128], F32, "grow")
    cp('dve', gT[:, 0, :], modT[:, 32:48, 0], [R_modT], [R_gT])
    cp('dve', gT[:, 1, :], modT[:, 80:96, 0], [R_modT], [R_gT])
    cp('dve', gT[:, 2:4, :], GS[:, 4:6, :], [R_GS], [R_gT])
    for g in range(4):
        tr(bank_f(0)[0:16, g * 128:(g + 1) * 128], gT[:, g, :], ident_f, [R_gT, R_idf], [PB[0]])
    cp('dve', grow, bank_f(0)[0:16, 0:512].rearrange("p (g n) -> p g n", g=4), [PB[0]], [R_grow])
    for g in range(4):
        dma('sp', gate_d[g].rearrange("(j p) -> j p", p=128), grow[:, g, :], [R_grow], (), 'c_gate', accw=[R_gate])

    w_in_v = w_in.rearrange("(kc p) n -> p kc n", p=128)
    kvcols = (1024, 2560, 3072, 3584, 4096)
    for i, c0 in enumerate(kvcols):
        wb_, R_wb = wst[i % 2]
        dma('sp', wb_, w_in_v[:, :, c0:c0 + 512], (), [R_wb], f'wst{i % 2}')
        cp('dve' if i % 2 == 0 else 'pool', wkv[:, :, i * 512:(i + 1) * 512], wb_, [R_wb], [R_wkv])

    if STOP == "A":
        dump(0, 0, modT.rearrange("p j r -> p (j r)"), R_modT, F32)
        dump(0, 192, GS.rearrange("p a b -> p (a b)"), R_GS, F32)
        dump(0, 288, ls, R_ls, F32)
        dump(0, 320, T2, R_T2, F32)
        return finish()

    def make_norm_bufs(n_in):
        bufs = {}
        bufs['xt'] = [A.alloc([128, D], F32, f"xt{i}") for i in range(n_in)]
        bufs['xs'] = [A.alloc([128, D], BF16, f"xs{i}") for i in range(2)]
        bufs['junk'] = A.alloc([128, D], BF16, "junk")[0]
        bufs['ss'] = [A.alloc([128, 1], F32, f"ss{i}") for i in range(2)]
        bufs['rs'] = [A.alloc([128, 1], F32, f"rs{i}") for i in range(2)]
        return bufs

    def norm_s1(bufs, i, src_ap, src_res, xs_out=None, pre=None):
        b = i % 2
        if src_res is None:
            xt, R_xt = bufs['xt'][b]
            dma('sp', xt, src_ap, (), [R_xt], f'xt{b}')
        else:
            xt, R_xt = src_ap, src_res
        ss, R_ss = bufs['ss'][b]
        rs, R_rs = bufs['rs'][b]
        xs, R_xs = bufs['xs'][b] if xs_out is None else xs_out
        mset('dve', ss, 0.0, [R_ss])
        junk = bufs['junk']
        actf(junk, xt, AF.Square, [R_xt, R_ss], [R_ss], accum_out=ss)
        rstd_ops('dve', rs, ss, 1.0 / D, [R_ss], [R_rs])
        actf(xs, xt, AF.Copy, [R_xt, R_rs], [R_xs], scale=rs[:, 0:1])
        return xs, R_xs

    def norm_s2(xs, R_xs, gi, hT_out, R_hT, pbank0):
        pT = bank_b(pbank0, 2)
        Rp = PB[pbank0]
        for j in range(16):
            tr(pT[:, j * 128:(j + 1) * 128], xs[:, j * 128:(j + 1) * 128], ident_b, [R_xs, R_idb], [Rp, PB[pbank0 + 1]])
        pv = pT.rearrange("p (j t) -> p j t", j=16)
        tt('dve', hT_out, pv, GS[:, gi, :].unsqueeze(2).to_broadcast([128, 16, 128]), ALU.mult,
           [Rp, PB[pbank0 + 1], R_GS], [R_hT])
        tt('dve', hT_out, hT_out, GS[:, gi + 1, :].unsqueeze(2).to_broadcast([128, 16, 128]), ALU.add,
           [R_hT, R_GS], [R_hT])

    def rope_ops(eng, src, R_src, rope_t, R_rope, nh, t1, R_t1, t2, R_t2, out, R_out):
        cos_b = rope_t[:, 0:128].unsqueeze(1).to_broadcast([128, nh, 128])
        tt(eng, t1, src, cos_b, ALU.mult, [R_src, R_rope], [R_t1])
        sv = src.rearrange("p h (a s i) -> p h a s i", a=2, s=2)
        tv = t2.rearrange("p h (a s i) -> p h a s i", a=2, s=2)
        sn = rope_t[:, 128:256].rearrange("p (a s i) -> p a s i", a=2, s=2)
        for s_ in range(2):
            tt(eng, tv[:, :, :, s_, :], sv[:, :, :, 1 - s_, :],
               sn[:, :, s_, :].unsqueeze(1).to_broadcast([128, nh, 2, 32]), ALU.mult, [R_src, R_rope], [R_t2])
        tt(eng, out, t1, t2, ALU.add, [R_t1, R_t2], [R_out])

    S.barrier()
    A.top = mark_b1
    nb = make_norm_bufs(2)
    gainK, R_gK = A.alloc([128, 10, 128], F32, "gainK")
    cp('pool', gainK[:, 0:2, :], gk_a.unsqueeze(1).to_broadcast([128, 2, 128]), [R_gka], [R_gK])
    cp('pool', gainK[:, 2:10, :], gk_b.unsqueeze(1).to_broadcast([128, 8, 128]), [R_gkb], [R_gK])
    hT = [A.alloc([128, 16, 128], BF16, f"hT{i}") for i in range(2)]
    ropet = [A.alloc([128, 256], F32, f"ropet{i}") for i in range(2)]
    vst = [A.alloc([128, 1280], BF16, f"vst{i}") for i in range(2)]
    sqk = [A.alloc([128, 1280], F32, f"sqk{i}") for i in range(2)]
    kss = [A.alloc([128, 10], F32, f"kss{i}") for i in range(2)]
    krs = [A.alloc([128, 10], F32, f"krs{i}") for i in range(2)]
    ksb = [A.alloc([128, 10, 128], F32, f"ksb{i}") for i in range(2)]
    kt1, R_kt1 = A.alloc([128, 10, 128], F32, "kt1")
    kt2, R_kt2 = A.alloc([128, 10, 128], F32, "kt2")
    kro = [A.alloc([128, 10, 128], BF16, f"kro{i}") for i in range(2)]
    kst = [A.alloc([128, 10, 256], BF16, f"kst{i}") for i in range(2)]
    KT_v = KT_d.rearrange("h d s -> d h s")
    xs_of = {}

    def b1_s1(t):
        src = ctx_in[t * 128:(t + 1) * 128, :] if t < 2 else x_all[(t - 2) * 128:(t - 1) * 128, :]
        xs_of[t] = norm_s1(nb, t, src, None)
        rp, R_rp = ropet[t % 2]
        dma('sp', rp, rope_all[t * 128:(t + 1) * 128, :], (), [R_rp], f'rp{t % 2}')

    def b1_s2(t):
        xs, R_xs = xs_of.pop(t)
        h, R_h = hT[t % 2]
        norm_s2(xs, R_xs, 2 if t < 2 else 0, h, R_h, 0)

    def b1_s3(t):
        h, R_h = hT[t % 2]
        for blk in range(5):
            for kc in range(16):
                mm(bank_f(2 + blk), h[:, kc, :], wkv[:, kc, blk * 512:(blk + 1) * 512], kc == 0, kc == 15,
                   [R_h, R_wkv], [PB[2 + blk]])

    def b1_s4(t):
        b = t % 2
        v, R_v = vst[b]
        cp('act', v[:, 0:256], bank_f(2)[:, 256:512], [PB[2]], [R_v])
        cp('act', v[:, 256:768], bank_f(5), [PB[5]], [R_v])
        cp('act', v[:, 768:1280], bank_f(6), [PB[6]], [R_v])
        dma('sp', V_d[t * 128:(t + 1) * 128, :], v, [R_v], (), f'vst{b}', accw=[R_V])
        sq, R_sq = sqk[b]
        actf(sq[:, 0:256], bank_f(2)[:, 0:256], AF.Square, [PB[2]], [R_sq])
        actf(sq[:, 256:768], bank_f(3), AF.Square, [PB[3]], [R_sq])
        actf(sq[:, 768:1280], bank_f(4), AF.Square, [PB[4]], [R_sq])
        ks, R_ks = kss[b]
        kr, R_kr = krs[b]
        red('dve', ks, sq.rearrange("p (h d) -> p h d", h=10), ALU.add, [R_sq], [R_ks])
        rstd_ops('dve', kr, ks, 1.0 / 128, [R_ks], [R_kr])
        kb_, R_kb = ksb[b]
        for (bk, h0, nh, c0) in ((2, 0, 2, 0), (3, 2, 4, 0), (4, 6, 4, 0)):
            tt('dve', kb_[:, h0:h0 + nh, :], bank_f(bk)[:, c0:c0 + nh * 128].rearrange("p (h d) -> p h d", h=nh),
               kr[:, h0:h0 + nh].unsqueeze(2).to_broadcast([128, nh, 128]), ALU.mult, [PB[bk], R_kr], [R_kb])
        tt('pool', kb_, kb_, gainK, ALU.mult, [R_kb, R_gK], [R_kb])
        rp, R_rp = ropet[b]
        ko, R_ko = kro[b]
        rope_ops('pool', kb_, R_kb, rp, R_rp, 10, kt1, R_kt1, kt2, R_kt2, ko, R_ko)

    def b1_s5(t):
        b = t % 2
        ko, R_ko = kro[b]
        gb_ = (t // 2) % 2
        slot = t % 2
        kq, R_kq = kst[gb_]
        for g in range(2):
            for h in range(5):
                tr(bank_b(7)[:, h * 128:(h + 1) * 128], ko[:, 5 * g + h, :], ident_b, [R_ko, R_idb], [PB[7]])
            cp('act', kq[:, 5 * g:5 * g + 5, slot * 128:(slot + 1) * 128],
               bank_b(7)[:, 0:640].rearrange("p (h t) -> p h t", h=5), [PB[7]], [R_kq])
        if slot == 1:
            dma('sp', KT_v[:, :, (t - 1) * 128:(t + 1) * 128], kq, [R_kq], (), f'kst{gb_}', accw=[R_KT])

    NT_RUN = int(os.environ.get('MK_NT', NST))
    for i in range(-2, NT_RUN + 1):
        if 0 <= i + 2 < NT_RUN:
            b1_s1(i + 2)
        if 0 <= i + 1 < NT_RUN:
            b1_s2(i + 1)
        if 0 <= i < NT_RUN:
            b1_s3(i)
            b1_s4(i)
        if 0 <= i - 1 < NT_RUN:
            b1_s5(i - 1)

    if STOP == "B1":
        S.barrier()
        A.top = mark_const
        nk = 2048 if NT_RUN == NST else 128 * NT_RUN
        for i_, (hh, s0) in enumerate(((0, 0), (2, 0), (9, 6400 if NT_RUN == NST else 0))):
            db, R_db = A.alloc([128, nk], BF16, f"db{i_}")
            dma('sp', db, KT_d[hh][:, s0:s0 + nk], [R_KT], [R_db], f'dbl{i_}')
            dump(128 * i_, 0, db, R_db, BF16)
        for i_, r0 in enumerate((0, 8320 if NT_RUN == NST else 128)):
            db, R_db = A.alloc([128, 1280], BF16, f"dv{i_}")
            dma('sp', db, V_d[r0:r0 + 128, :], [R_V], [R_db], f'dvl{i_}')
            dump(384 + 128 * i_, 0, db, R_db, BF16)
        return finish()

    S.barrier()
    A.top = mark_const
    topoff = A.n2 - 2 * 8 * TOK
    oaT = A.base[:, topoff:topoff + 8 * TOK].rearrange("p (a b) -> p a b", a=8)
    obT = A.base[:, topoff + 8 * TOK:topoff + 16 * TOK].rearrange("p (a b) -> p a b", a=8)
    R_oaT = Res("oaT")
    R_obT = Res("obT")
    qT, R_qT = A.alloc([128, 16, TOK], BF16, "qT")
    mark_c = A.top
    hTo, R_hTo = A.alloc([128, 16, TOK], BF16, "hT_own")
    nb = make_norm_bufs(2)
    ropeo, R_ropeo = A.alloc([128, NTT, 256], F32, "ropeo")
    dma('sp', ropeo, rope_own.rearrange("(t p) c -> p t c", p=128), (), [R_ropeo], 'c_ropeo')
    wq = [A.alloc([128, 16, 512], F32, f"wq{i}") for i in range(2)]
    sqq, R_sqq = A.alloc([128, 512], F32, "sqq")
    qss = [A.alloc([128, 4], F32, f"qss{i}") for i in range(2)]
    qrs = [A.alloc([128, 4], F32, f"qrs{i}") for i in range(2)]
    qsb = [A.alloc([128, 4, 128], F32, f"qsb{i}") for i in range(2)]
    qt1, R_qt1 = A.alloc([128, 4, 128], F32, "qt1")
    qt2, R_qt2 = A.alloc([128, 4, 128], F32, "qt2")
    qro = [A.alloc([128, 4, 128], BF16, f"qro{i}") for i in range(2)]
    sgst = [A.alloc([128, 512], BF16, f"sgst{i}") for i in range(2)]

    for t_ in range(NTT + 1):
        if t_ < NTT:
            xs_of[t_] = norm_s1(nb, t_, x_own[t_ * 128:(t_ + 1) * 128, :], None)
        if t_ >= 1:
            xs, R_xs = xs_of.pop(t_ - 1)
            norm_s2(xs, R_xs, 0, hTo[:, :, (t_ - 1) * 128:t_ * 128], R_hTo, 0)

    wqi = 0
    for qb in range(4):
        c0 = qb * 512 if qb < 2 else 1536 + (qb - 2) * 512
        gq, R_gq = (gq_a, R_gqa) if qb < 2 else (gq_b, R_gqb)
        w_, R_w = wq[wqi % 2]
        dma('sp', w_, w_in_v[:, :, c0:c0 + 512], (), [R_w], f'wq{wqi % 2}')
        wqi += 1
        wv = bfv(w_)

        def q_front(t_):
            pb = 2 + t_ % 2
            i2 = t_ % 2
            for kc in range(16):
                mm(bank_f(pb), hTo[:, kc, t_ * 128:(t_ + 1) * 128], wv[:, kc, :], kc == 0, kc == 15,
                   [R_hTo, R_w], [PB[pb]])
            actf(sqq, bank_f(pb), AF.Square, [PB[pb]], [R_sqq])
            red('dve', qss[i2][0], sqq.rearrange("p (h d) -> p h d", h=4), ALU.add, [R_sqq], [qss[i2][1]])
            rstd_ops('dve', qrs[i2][0], qss[i2][0], 1.0 / 128, [qss[i2][1]], [qrs[i2][1]])
            qs_, R_qs = qsb[i2]
            tt('dve', qs_, bank_f(pb).rearrange("p (h d) -> p h d", h=4),
               qrs[i2][0].unsqueeze(2).to_broadcast([128, 4, 128]), ALU.mult, [PB[pb], qrs[i2][1]], [R_qs])
            tt('pool', qs_, qs_, gq.unsqueeze(1).to_broadcast([128, 4, 128]), ALU.mult, [R_qs, R_gq], [R_qs])
            rope_ops('pool', qs_, R_qs, ropeo[:, t_, :], R_ropeo, 4, qt1, R_qt1, qt2, R_qt2, qro[i2][0], qro[i2][1])

        def q_back(t_):
            i2 = t_ % 2
            for h in range(4):
                tr(bank_b(4)[:, h * 128:(h + 1) * 128], qro[i2][0][:, h, :], ident_b, [qro[i2][1], R_idb], [PB[4]])
            cp('act', qT[:, 4 * qb:4 * qb + 4, t_ * 128:(t_ + 1) * 128],
               bank_b(4)[:, 0:512].rearrange("p (h t) -> p h t", h=4), [PB[4]], [R_qT])

        for t_ in range(NTT + 1):
            if t_ < NTT:
                q_front(t_)
            if t_ >= 1:
                q_back(t_ - 1)

    cnt = 0
    for gbk in range(8):
        c0 = 4608 + gbk * 512
        w_, R_w = wq[wqi % 2]
        dma('sp', w_, w_in_v[:, :, c0:c0 + 512], (), [R_w], f'wq{wqi % 2}')
        wqi += 1
        wv = bfv(w_)
        for ch in range(4):
            for half in range(2):
                pb = 5 + cnt % 2
                for kc in range(16):
                    mm(bank_f(pb), wv[:, kc, ch * 128:(ch + 1) * 128], hTo[:, kc, half * 512:(half + 1) * 512],
                       kc == 0, kc == 15, [R_hTo, R_w], [PB[pb]])
                sg_, R_sg = sgst[cnt % 2]
                actf(sg_, bank_f(pb), AF.Sigmoid, [PB[pb]], [R_sg])
                dma('sp', SG_d[4 * gbk + ch][:, half * 512:(half + 1) * 512], sg_, [R_sg], (), f'sg{cnt % 2}',
                    accw=[R_SG])
                cnt += 1

    S.barrier()
    A.top = mark_c
    A.limit = topoff
    zt, R_zt = A.alloc([128, D], BF16, "zt")
    mset('dve', zt, 0.0, [R_zt])
    ostg_g, R_og = A.alloc([128, 4, 128], BF16, "ostg_g")
    ostg_d, R_od = A.alloc([128, 4, 256], BF16, "ostg_d")
    kbuf = [A.alloc([128, NKEY], BF16, f"kbuf{i}") for i in range(2)]
    vraw = [A.alloc([128, NST * 257], BF16, f"vraw{i}") for i in range(2)]
    ebuf = [A.alloc([128, 512], BF16, f"ebuf{i}") for i in range(3)]
    obf = [A.alloc([128, 4, 256], F32, f"obf{i}") for i in range(2)]
    osq, R_osq = A.alloc([128, 4, 256], F32, "osq")
    rec, R_rec = A.alloc([128, 4], F32, "rec")
    rec2, R_rec2 = A.alloc([128, 4], F32, "rec2")
    ssb, R_ssb = A.alloc([128, 4], F32, "ssb")
    rsb, R_rsb = A.alloc([128, 4], F32, "rsb")
    V_v = V_d.rearrange("(t p) c -> p t c", p=128)
    kcnt = [0]
    vcnt = [0]

    NSA = NT_RUN

    def load_k(idx):
        i = kcnt[0] % 2
        kcnt[0] += 1
        kb_, R_kb = kbuf[i]
        dma('sp', kb_[:, 0:NSA * 128], KT_d[idx][:, 0:NSA * 128], [R_KT], [R_kb], f'kb{i}')
        return kb_, R_kb

    def load_v(c0, dv):
        i = vcnt[0] % 2
        vcnt[0] += 1
        raw, R_raw = vraw[i]
        view = raw[:, 0:NST * (dv + 1)].rearrange("p (t c) -> p t c", c=dv + 1)
        mset('dve', view[:, 0:NSA, dv:dv + 1], 1.0, [R_raw])
        dma('sp', view[:, 0:NSA, 0:dv], V_v[:, 0:NSA, c0:c0 + dv], [R_V], [R_raw], f'vb{i}')
        return view, R_raw

    po_banks = (3, 4, 5, 6)

    def attn_block(kb_, R_kb, vv, R_v, dv, qh, qb):
        def Sm(st):
            mm(bank_f(st % 3), kb_[:, st * 128:(st + 1) * 128], qT[:, qh, qb * 512:(qb + 1) * 512], True, True,
               [R_kb, R_qT], [PB[st % 3]])

        def Ex(st):
            e_, R_e = ebuf[st % 3]
            actf(e_, bank_f(st % 3), AF.Exp, [PB[st % 3]], [R_e], scale=SCALE)

        def PV(st):
            e_, R_e = ebuf[st % 3]
            for q4 in range(4):
                mm(bank_f(po_banks[q4])[:, 0:dv + 1], e_[:, q4 * 128:(q4 + 1) * 128], vv[:, st, :],
                   st == 0, st == NSA - 1, [R_e, R_v], [PB[po_banks[q4]]])
        Sm(0)
        Sm(1)
        for st in range(NSA):
            Ex(st)
            if st + 2 < NSA:
                Sm(st + 2)
            PV(st)

    jobs = [('g', 0), ('g', 1), ('d', 0), ('d', 1), ('d', 2), ('d', 3)]

    def job_v(job):
        kind, i = job
        if kind == 'g':
            return load_v(i * 128, 128)
        return load_v(256 + i * 256, 256)

    def job_k(job):
        kind, i = job
        if kind == 'g':
            return [load_k(i)]
        return [load_k(2 + 2 * i), load_k(2 + 2 * i + 1)]

    loaded_v = job_v(jobs[0])
    for ji, job in enumerate(jobs):
        cur_v = loaded_v
        ks_ = job_k(job)
        if ji + 1 < len(jobs):
            loaded_v = job_v(jobs[ji + 1])
        if ji == 0:
            for r0 in range(0, NSLOT, 128):
                nr_ = min(128, NSLOT - r0)
                dma('pool', Xg_d[r0:r0 + nr_, :], zt[0:nr_, :], [R_zt], (), 'zfill', accw=[R_Xg])
                if int(os.environ.get('MK_NRANK', NEXP)) < NEXP:
                    dma('pool', Yg_d[r0:r0 + nr_, :], zt[0:nr_, :], [R_zt], (), 'zfill', accw=[R_Yg])
            dma('pool', Xg_d[NSLOT:NSLOT + 1, :], zt[0:1, :], [R_zt], (), 'zfill', accw=[R_Xg])
            dma('pool', Yg_d[NSLOT:NSLOT + 1, :], zt[0:1, :], [R_zt], (), 'zfill', accw=[R_Yg])
        cur = (ks_, cur_v)
        kind, i = job
        ks_, (vv, R_v) = cur
        if kind == 'g':
            kb_, R_kb = ks_[0]
            for qh in range(4 * i, 4 * i + 4):
                for qb in range(2):
                    attn_block(kb_, R_kb, vv, R_v, 128, qh, qb)
                    for q4 in range(4):
                        pb = po_banks[q4]
                        S.op('dve', (lambda o_, i_: lambda e: e.reciprocal(out=o_, in_=i_))(rec[:, q4:q4 + 1], bank_f(pb)[:, 128:129]),
                             [PB[pb]], [R_rec])
                        ts('dve', ostg_g[:, q4, :], bank_f(pb)[:, 0:128],
                           rec[:, q4:q4 + 1], None, ALU.mult, None, [PB[pb], R_rec], [R_og])
                    for q4 in range(4):
                        tr(bank_b(7)[:, q4 * 128:(q4 + 1) * 128], ostg_g[:, q4, :], ident_b, [R_og, R_idb], [PB[7]])
                    cp('dve', oaT[:, qh, qb * 512:(qb + 1) * 512], bank_b(7)[:, 0:512], [PB[7]], [R_oaT])
        else:
            for qb in range(2):
                of_, R_of = obf[qb % 2]
                for m in range(2):
                    kb_, R_kb = ks_[m]
                    attn_block(kb_, R_kb, vv, R_v, 256, 8 + 2 * i + m, qb)
                    for q4 in range(4):
                        pb = po_banks[q4]
                        if m == 0:
                            S.op('dve', (lambda o_, i_: lambda e: e.reciprocal(out=o_, in_=i_))(rec[:, q4:q4 + 1], bank_f(pb)[:, 256:257]),
                                 [PB[pb]], [R_rec])
                            ts('dve', of_[:, q4, :], bank_f(pb)[:, 0:256], rec[:, q4:q4 + 1], None, ALU.mult, None,
                               [PB[pb], R_rec], [R_of])
                        else:
                            S.op('dve', (lambda o_, i_: lambda e: e.reciprocal(out=o_, in_=i_))(rec2[:, q4:q4 + 1], bank_f(pb)[:, 256:257]),
                                 [PB[pb]], [R_rec2])
                            ts('dve', rec2[:, q4:q4 + 1], rec2[:, q4:q4 + 1], neglam[:, 0:1], None, ALU.mult, None,
                               [R_rec2, R_nl], [R_rec2])
                            stt('dve', of_[:, q4, :], bank_f(pb)[:, 0:256], rec2[:, q4:q4 + 1], of_[:, q4, :],
                                ALU.mult, ALU.add, [PB[pb], R_rec2, R_of], [R_of])
                tt('dve', osq, of_, of_, ALU.mult, [R_of], [R_osq])
                red('dve', ssb, osq, ALU.add, [R_osq], [R_ssb])
                rstd_ops('dve', rsb, ssb, 1.0 / 256, [R_ssb], [R_rsb])
                tt('dve', of_, of_, rsb.unsqueeze(2).to_broadcast([128, 4, 256]), ALU.mult, [R_of, R_rsb], [R_of])
                tt('dve', ostg_d, of_,
                   gsub.unsqueeze(1).to_broadcast([128, 4, 256]), ALU.mult, [R_of, R_gsub], [R_od])
                for c_ in range(2):
                    for q4 in range(4):
                        tr(bank_b(7)[:, (c_ * 4 + q4) * 128:(c_ * 4 + q4 + 1) * 128],
                           ostg_d[:, q4, c_ * 128:(c_ + 1) * 128], ident_b, [R_od, R_idb], [PB[7]])
                cp('dve', obT[:, 2 * i:2 * i + 2, qb * 512:(qb + 1) * 512],
                   bank_b(7).rearrange("p (c t) -> p c t", c=2), [PB[7]], [R_obT])

    if STOP == "C":
        S.barrier()
        return finish()

    S.barrier()
    A.top = mark_const
    yT, R_yT = A.alloc([128, 16, TOK], BF16, "yT")
    mark_d2 = A.top
    wab = [A.alloc([128, 8, 256], F32, f"wab{i}") for i in range(4)]
    sgl = [A.alloc([128, 2, TOK], BF16, f"sgl{i}") for i in range(2)]
    ytm = [A.alloc([128, 512], F32, f"ytm{i}") for i in range(4)]
    wa_v = w_ba.rearrange("(ac p) n -> p ac n", p=128)
    wb_v = w_bb.rearrange("(ac p) n -> p ac n", p=128)
    k = 0
    for cb in range(8):
        wa_, R_wa = wab[cb % 2]
        wb2, R_wb2 = wab[2 + cb % 2]
        dma('sp', wa_, wa_v[:, :, cb * 256:(cb + 1) * 256], (), [R_wa], f'wab{cb % 2}')
        dma('sp', wb2, wb_v[:, :, cb * 256:(cb + 1) * 256], (), [R_wb2], f'wab{2 + cb % 2}')
        wav = bfv(wa_)
        wbv = bfv(wb2)
        for dcl in range(2):
            dc = 2 * cb + dcl
            sg_, R_sg = sgl[dc % 2]
            dma('sp', sg_[:, 0, :], SG_d[dc], [R_SG], [R_sg], f'sgl{dc % 2}')
            dma('sp', sg_[:, 1, :], SG_d[16 + dc], [R_SG], [R_sg], f'sgl{dc % 2}')
            for half in range(2):
                pa = k % 2
                pbk = 2 + k % 2
                hs = slice(half * 512, (half + 1) * 512)
                for ac in range(8):
                    mm(bank_f(pa), wav[:, ac, dcl * 128:(dcl + 1) * 128], oaT[:, ac, hs], ac == 0, ac == 7,
                       [R_wa, R_oaT], [PB[pa]])
                for ac in range(8):
                    mm(bank_f(pbk), wbv[:, ac, dcl * 128:(dcl + 1) * 128], obT[:, ac, hs], ac == 0, ac == 7,
                       [R_wb2, R_obT], [PB[pbk]])
                ya, R_ya = ytm[(2 * k) % 4]
                yb, R_yb = ytm[(2 * k + 1) % 4]
                tt('dve', ya, bank_f(pa), sg_[:, 0, hs], ALU.mult, [PB[pa], R_sg], [R_ya])
                tt('dve', yb, bank_f(pbk), sg_[:, 1, hs], ALU.mult, [PB[pbk], R_sg], [R_yb])
                tt('pool', yT[:, dc, hs], ya, yb, ALU.add, [R_ya, R_yb], [R_yT])
                k += 1

    S.barrier()
    A.top = mark_d2
    A.limit = A.n2
    x1, R_x1 = A.alloc([128, NTT, D], F32, "x1")
    dma('sp', x1, x_own.rearrange("(t p) d -> p t d", p=128), (), [R_x1], 'x1ld')
    wo = [A.alloc([128, 16, 256], F32, f"wo{i}") for i in range(2)]
    gateB, R_gB = A.alloc([128, D], F32, "gateB")
    otm = [A.alloc([128, 512], F32, f"otm{i}") for i in range(2)]
    dma('sp', gateB, gate_d[0].partition_broadcast(128), [R_gate], [R_gB], 'c_gB')
    wo_v = w_out.rearrange("(dc p) n -> p dc n", p=128)
    k = 0
    for cb in range(8):
        w_, R_w = wo[cb % 2]
        dma('sp', w_, wo_v[:, :, cb * 256:(cb + 1) * 256], (), [R_w], f'wo{cb % 2}')
        wv = bfv(w_)
        cs = slice(cb * 256, (cb + 1) * 256)
        for t_ in range(NTT):
            pb = k % 2
            for dc in range(16):
                mm(bank_f(pb)[:, 0:256], yT[:, dc, t_ * 128:(t_ + 1) * 128], wv[:, dc, :], dc == 0, dc == 15,
                   [R_yT, R_w], [PB[pb]])
            o_, R_o = otm[k % 2]
            o_ = o_[:, 0:256]
            tt('dve', o_, bank_f(pb)[:, 0:256], gateB[:, cs], ALU.mult, [PB[pb], R_gB], [R_o])
            tt('pool', x1[:, t_, cs], x1[:, t_, cs], o_, ALU.add, [R_x1, R_o], [R_x1])
            k += 1

    X1_d = nc.dram_tensor("X1_d", [TOK, D], F32).ap()
    R_X1d = Res("X1_d")
    dma('sp', X1_d.rearrange("(t p) d -> p t d", p=128), x1, [R_x1], [R_X1d], 'x1st')
    S.barrier()
    A.top = mark_const
    X2_d = nc.dram_tensor("X2_d", [TOK, D], F32).ap()
    R_X2d = Res("X2_d")
    slw, R_slw = A.alloc([128, NTT, 2, 8], F32, "slw")
    slots_i, R_sli = A.alloc([128, NTT, 8], I32, "slots_i")
    idxw, R_idxw = A.alloc([128, 2, 64], I32, "idxw")
    mark_keep = A.top
    h2T, R_h2T = A.alloc([128, 16, TOK], BF16, "h2T")
    mark_h2 = A.top
    xs2, R_xs2 = A.alloc([128, NTT, D], BF16, "xs2")
    wn, R_wn = A.alloc([128, NTT, 64], F32, "wn")
    selb, R_selb = A.alloc([128, NTT, 64], BF16, "selb")
    bmk_all, R_bmka = A.alloc([128, NTT, 64], F32, "bmk_all")
    t8_all, R_t8a = A.alloc([128, NTT, 8], F32, "t8_all")
    tri_f, R_trif = A.alloc([128, 128], F32, "tri_f")
    tri_b, R_trib = A.alloc([128, 128], BF16, "tri_b")
    dma('sp', tri_f, tri_in, (), [R_trif], 'c_tri')
    cp('dve', tri_b, tri_f, [R_trif], [R_trib])
    nb = make_norm_bufs(2)
    wr, R_wr = A.alloc([128, 16, NEXP], F32, "wr")
    dma('sp', wr, w_router.rearrange("(kc p) e -> p kc e", p=128), (), [R_wr], 'c_wr')
    wrv = bfv(wr)
    rt = {}
    for nm, shp in (('sc', [128, 512]), ('bi', [128, 512]), ('m1', [128, 64]), ('eq', [128, 512]), ('bm', [128, 512]),
                    ('m2', [128, 64]), ('gs', [128, 64]), ('cmp', [128, 512]), ('rank', [128, 64]), ('gm', [128, 64]),
                    ('thr', [128, 8]), ('sel', [128, 512]), ('ws', [128, 512]), ('wsum', [128, 8]),
                    ('sf', [128, 512]), ('oh', [128, 512]), ('pr', [128, 512])):
        rt[nm] = A.alloc(shp, F32, "rt_" + nm)
    for t_ in range(NTT + 1):
        if t_ < NTT:
            xs_of[t_] = norm_s1(nb, t_, X1_d[t_ * 128:(t_ + 1) * 128, :], None, xs_out=(xs2[:, t_, :], R_xs2))
        if t_ >= 1:
            xs, R_xs = xs_of.pop(t_ - 1)
            norm_s2(xs, R_xs, 4, h2T[:, :, (t_ - 1) * 128:t_ * 128], R_h2T, 0)

    GSB, R_GSB = A.alloc([128, 2, D], F32, "GSB")
    dma('sp', GSB[:, 0, :], gate_d[2].partition_broadcast(128), [R_gate], [R_GSB], 'c_gsb')
    dma('sp', GSB[:, 1, :], gate_d[3].partition_broadcast(128), [R_gate], [R_GSB], 'c_gsb')
    for t_ in range(NTT):
        tt('pool', xs2[:, t_, :], xs2[:, t_, :], GSB[:, 0, :], ALU.mult, [R_xs2, R_GSB], [R_xs2])
        tt('pool', xs2[:, t_, :], xs2[:, t_, :], GSB[:, 1, :], ALU.add, [R_xs2, R_GSB], [R_xs2])

    def v3(nm):
        return rt[nm][0].rearrange("p (t e) -> p t e", t=NTT)

    def v4(nm):
        return rt[nm][0].rearrange("p (q i) -> p q i", i=8)

    def vq(nm):
        return rt[nm][0].rearrange("p (t g) -> p t g", t=NTT)
    for t_ in range(NTT):
        for kc in range(16):
            mm(bank_f(2)[:, t_ * 64:(t_ + 1) * 64], h2T[:, kc, t_ * 128:(t_ + 1) * 128], wrv[:, kc, :],
               kc == 0, kc == 15, [R_h2T, R_wr], [PB[2]])
    sc, R_sc = rt['sc']
    bi, R_bi = rt['bi']
    actf(sc, bank_f(2), AF.Sigmoid, [PB[2]], [R_sc])
    tt('dve', v3('bi'), v3('sc'), rbias.unsqueeze(1).to_broadcast([128, NTT, 64]), ALU.add, [R_sc, R_rb], [R_bi])
    S.op('dve', lambda e: e.tensor_reduce(out=rt['m1'][0], in_=v4('bi'), axis=AX.X, op=ALU.max), [R_bi], [rt['m1'][1]])
    tt('dve', v4('eq'), v4('bi'), rt['m1'][0].unsqueeze(2).to_broadcast([128, 64, 8]), ALU.is_equal,
       [R_bi, rt['m1'][1]], [rt['eq'][1]])
    stt('dve', rt['bm'][0], rt['eq'][0], -1e9, bi, ALU.mult, ALU.add, [rt['eq'][1], R_bi], [rt['bm'][1]])
    S.op('dve', lambda e: e.tensor_reduce(out=rt['m2'][0], in_=v4('bm'), axis=AX.X, op=ALU.max), [rt['bm'][1]], [rt['m2'][1]])
    tt('dve', rt['gs'][0], rt['m1'][0], rt['m2'][0], ALU.add, [rt['m1'][1], rt['m2'][1]], [rt['gs'][1]])
    gq = vq('gs')
    cmp4 = rt['cmp'][0].rearrange("p (t g h) -> p t g h", t=NTT, g=8)
    tt('dve', cmp4, gq.unsqueeze(2).to_broadcast([128, NTT, 8, 8]), gq.unsqueeze(3).to_broadcast([128, NTT, 8, 8]),
       ALU.is_gt, [rt['gs'][1]], [rt['cmp'][1]])
    S.op('dve', lambda e: e.tensor_reduce(out=rt['rank'][0], in_=v4('cmp'), axis=AX.X, op=ALU.add), [rt['cmp'][1]], [rt['rank'][1]])
    ts('dve', rt['gm'][0], rt['rank'][0], 3.5, None, ALU.is_lt, None, [rt['rank'][1]], [rt['gm'][1]])
    ts('dve', rt['gm'][0], rt['gm'][0], -1.0, 1e9, ALU.add, ALU.mult, [rt['gm'][1]], [rt['gm'][1]])
    bmk4 = bmk_all.rearrange("p t (g i) -> p (t g) i", i=8)
    tt('dve', bmk4, v4('bi'), rt['gm'][0].unsqueeze(2).to_broadcast([128, 64, 8]), ALU.add,
       [R_bi, rt['gm'][1]], [R_bmka])
    for t_ in range(NTT):
        S.op('dve', (lambda o_, i_: lambda e: e.max(out=o_, in_=i_))(t8_all[:, t_, :], bmk_all[:, t_, :]),
             [R_bmka], [R_t8a])
    S.op('dve', lambda e: e.tensor_reduce(out=rt['thr'][0], in_=t8_all, axis=AX.X, op=ALU.min), [R_t8a], [rt['thr'][1]])
    tt('dve', v3('sel'), bmk_all, rt['thr'][0].unsqueeze(2).to_broadcast([128, NTT, 64]), ALU.is_ge,
       [R_bmka, rt['thr'][1]], [rt['sel'][1]])
    cp('dve', selb, v3('sel'), [rt['sel'][1]], [R_selb])
    tt('dve', rt['ws'][0], sc, rt['sel'][0], ALU.mult, [R_sc, rt['sel'][1]], [rt['ws'][1]])
    red('dve', rt['wsum'][0], v3('ws'), ALU.add, [rt['ws'][1]], [rt['wsum'][1]])
    S.op('dve', lambda e: e.reciprocal(out=rt['wsum'][0], in_=rt['wsum'][0]), [rt['wsum'][1]], [rt['wsum'][1]])
    stt('dve', wn, v3('ws'), 2.5, rt['wsum'][0].unsqueeze(2).to_broadcast([128, NTT, 64]), ALU.mult, ALU.mult,
        [rt['ws'][1], rt['wsum'][1]], [R_wn])

    cnt, R_cnt = A.alloc([128, 64], F32, "cnt")
    rank, R_rank = A.alloc([128, 64], F32, "rank")
    ebase, R_ebase = A.alloc([128, 64], F32, "ebase")
    pif, R_pif = A.alloc([128, 64], F32, "pif")
    c3a, R_c3a = A.alloc([128, 16, 64], F32, "c3a")
    c3b, R_c3b = A.alloc([128, 16, 64], F32, "c3b")
    ltc, R_ltc = A.alloc([128, 16, 64], F32, "ltc")
    ctab, R_ctab = A.alloc([128, 3, 64], F32, "ctab")
    dma('sp', ctab, ctab_in.rearrange("p (a b) -> p a b", a=3), (), [R_ctab], 'c_ctab')
    iota64 = ctab[:, 0, :]
    btab = ctab[:, 1, :]
    for t2 in range(NTT):
        mm(bank_f(6)[:, 0:64], ones_b, selb[:, t2, :], t2 == 0, t2 == NTT - 1, [R_ones, R_selb], [PB[6]])
    cp('dve', cnt, bank_f(6)[:, 0:64], [PB[6]], [R_cnt])
    cnt_e2 = cnt.unsqueeze(1).to_broadcast([128, 16, 64])
    for ch in range(4):
        cs_ = slice(ch * 16, ch * 16 + 16)
        dma('sp', ltc, lt_in[:, ch * 1024:(ch + 1) * 1024].rearrange("p (a b) -> p a b", a=16), (), [R_ltc], 'c_ltc')
        cnt_e = cnt[:, cs_].unsqueeze(2).to_broadcast([128, 16, 64])
        tt('dve', c3a, cnt_e2, cnt_e, ALU.is_gt, [R_cnt], [R_c3a])
        tt('dve', c3b, cnt_e2, cnt_e, ALU.is_equal, [R_cnt], [R_c3b])
        tt('dve', c3b, c3b, ltc, ALU.mult, [R_c3b, R_ltc], [R_c3b])
        tt('dve', c3a, c3a, c3b, ALU.add, [R_c3a, R_c3b], [R_c3a])
        red('dve', rank[:, cs_], c3a, ALU.add, [R_c3a], [R_rank])
    for ch in range(4):
        cs_ = slice(ch * 16, ch * 16 + 16)
        tt('dve', c3a, rank[:, cs_].unsqueeze(2).to_broadcast([128, 16, 64]),
           iota64.unsqueeze(1).to_broadcast([128, 16, 64]), ALU.is_equal, [R_rank, R_ctab], [R_c3a])
        tt('dve', c3a, c3a, btab.unsqueeze(1).to_broadcast([128, 16, 64]), ALU.mult, [R_c3a, R_ctab], [R_c3a])
        red('dve', ebase[:, cs_], c3a, ALU.add, [R_c3a], [R_ebase])
        tt('dve', c3b, rank.unsqueeze(1).to_broadcast([128, 16, 64]),
           iota64[:, cs_].unsqueeze(2).to_broadcast([128, 16, 64]), ALU.is_equal, [R_rank, R_ctab], [R_c3b])
        tt('dve', c3b, c3b, iota64.unsqueeze(1).to_broadcast([128, 16, 64]), ALU.mult, [R_c3b, R_ctab], [R_c3b])
        red('dve', pif[:, cs_], c3b, ALU.add, [R_c3b], [R_pif])
    ts('dve', pif, pif, 128.0, ctab[:, 2, 0:1], ALU.mult, ALU.add, [R_pif, R_ctab], [R_pif])
    ts('dve', pif, pif, 2.0, None, ALU.mult, None, [R_pif], [R_pif])
    cp('dve', idxw[:, 0, :], pif, [R_pif], [R_idxw])
    ts('dve', pif, pif, 1.0, None, ALU.add, None, [R_pif], [R_pif])
    cp('dve', idxw[:, 1, :], pif, [R_pif], [R_idxw])

    for t_ in range(NTT):
        pp = bank_f(4)[:, t_ * 64:(t_ + 1) * 64]
        for t2 in range(t_ + 1):
            mm(pp, ones_b if t2 < t_ else tri_b, selb[:, t2, :], t2 == 0, t2 == t_,
               [R_ones, R_trib, R_selb], [PB[4]])
    sf3 = v3('sf')
    tt('dve', sf3, bank_f(4).rearrange("p (t e) -> p t e", t=NTT), ebase.unsqueeze(1).to_broadcast([128, NTT, 64]),
       ALU.add, [PB[4], R_ebase], [rt['sf'][1]])
    ts('dve', rt['sf'][0], rt['sf'][0], -float(TRASH), None, ALU.add, None, [rt['sf'][1]], [rt['sf'][1]])
    tt('dve', rt['sf'][0], rt['sf'][0], rt['sel'][0], ALU.mult, [rt['sf'][1], rt['sel'][1]], [rt['sf'][1]])
    ts('dve', rt['sf'][0], rt['sf'][0], float(TRASH), None, ALU.add, None, [rt['sf'][1]], [rt['sf'][1]])
    for k_ in range(8):
        tt('dve', v3('oh'), bmk_all, t8_all[:, :, k_:k_ + 1].to_broadcast([128, NTT, 64]), ALU.is_equal,
           [R_bmka, R_t8a], [rt['oh'][1]])
        tt('dve', rt['pr'][0], rt['oh'][0], rt['sf'][0], ALU.mult, [rt['oh'][1], rt['sf'][1]], [rt['pr'][1]])
        red('dve', slw[:, :, 0, k_], v3('pr'), ALU.add, [rt['pr'][1]], [R_slw])
        tt('dve', v3('pr'), v3('oh'), wn, ALU.mult, [rt['oh'][1], R_wn], [rt['pr'][1]])
        red('dve', slw[:, :, 1, k_], v3('pr'), ALU.add, [rt['pr'][1]], [R_slw])
    cp('dve', slots_i, slw[:, :, 0, :], [R_slw], [R_sli])

    def scatter(t_, k_):
        off = bass.IndirectOffsetOnAxis(ap=slots_i[:, t_, k_:k_ + 1], axis=0)
        src = xs2[:, t_, :]
        S.dma('pool', lambda e: e.indirect_dma_start(out=Xg_d[:, :], out_offset=off, in_=src, in_offset=None),
              [R_xs2, R_sli], (), 'scat', accw=[R_Xg])
    for t_ in range(NTT):
        for k_ in range(8):
            scatter(t_, k_)

    S.barrier()
    A.top = mark_h2
    x1, R_x1 = A.alloc([128, NTT, D], F32, "acc")
    ring = [A.alloc([128, 4096], F32, f"ring{i}") for i in range(4)]
    actTd, R_ad = A.alloc([128, 4, TOK], BF16, "actTd")
    sil = [A.alloc([128, 512], F32, f"sil{i}") for i in range(2)]
    gateB2, R_gB2 = A.alloc([128, D], F32, "gateB2")
    dma('sp', gateB2, gate_d[1].partition_broadcast(128), [R_gate], [R_gB2], 'c_gB2')
    rcnt = [0]

    def ring_load(src_ap, shape3):
        i = rcnt[0] % 4
        rcnt[0] += 1
        r_, R_r = ring[i]
        view = r_.rearrange("p (a b) -> p a b", a=shape3[0])
        dma('sp', view, src_ap, (), [R_r], f'ring{i}')
        return view, R_r

    def shared_expert(wg_ap, wu_ap, wd_ap):
        a_, R_a = actTd, R_ad
        wgv = wg_ap.rearrange("(kc p) f -> p kc f", p=128)
        wuv = wu_ap.rearrange("(kc p) f -> p kc f", p=128)
        wdv = wd_ap.rearrange("(fc p) d -> p fc d", p=128)
        kk = 0
        for pair in range(2):
            g_, R_g = ring_load(wgv[:, :, pair * 256:(pair + 1) * 256], [16, 256])
            u_, R_u = ring_load(wuv[:, :, pair * 256:(pair + 1) * 256], [16, 256])
            gv = bfv(g_)
            uv = bfv(u_)
            for fcl in range(2):
                fc = 2 * pair + fcl
                for half in range(2):
                    pg = (2 * kk) % 4
                    pu = (2 * kk + 1) % 4
                    hs = slice(half * 512, (half + 1) * 512)
                    for kc in range(16):
                        mm(bank_f(pg), gv[:, kc, fcl * 128:(fcl + 1) * 128], h2T[:, kc, hs], kc == 0, kc == 15,
                           [R_g, R_h2T], [PB[pg]])
                    for kc in range(16):
                        mm(bank_f(pu), uv[:, kc, fcl * 128:(fcl + 1) * 128], h2T[:, kc, hs], kc == 0, kc == 15,
                           [R_u, R_h2T], [PB[pu]])
                    s_, R_s = sil[kk % 2]
                    actf(s_, bank_f(pg), AF.Silu, [PB[pg]], [R_s])
                    tt('dve', a_[:, fc, hs], bank_f(pu), s_, ALU.mult, [PB[pu], R_s], [R_a])
                    kk += 1
        d0, R_d0 = ring_load(wdv[:, 0:2, :], [2, 2048])
        d1, R_d1 = ring_load(wdv[:, 2:4, :], [2, 2048])
        dv0 = bfv(d0)
        dv1 = bfv(d1)
        kk = 0
        for t_ in range(NTT):
            for cb in range(4):
                pb = 4 + kk % 4
                cs = slice(cb * 512, (cb + 1) * 512)
                for fc in range(4):
                    dvv, R_dd = (dv0, R_d0) if fc < 2 else (dv1, R_d1)
                    mm(bank_f(pb), a_[:, fc, t_ * 128:(t_ + 1) * 128], dvv[:, fc % 2, cs], fc == 0, fc == 3,
                       [R_a, R_dd], [PB[pb]])
                tt('dve', x1[:, t_, cs], bank_f(pb), gateB2[:, cs], ALU.mult, [PB[pb], R_gB2], [R_x1])
                kk += 1
            xr_, R_xr = xrs[0]
            dma('sp', xr_, X1_d[t_ * 128:(t_ + 1) * 128, :], [R_X1d], [R_xr], 'xrs0')
            tt('pool', xr_, xr_, x1[:, t_, :], ALU.add, [R_xr, R_x1], [R_xr])
            dma('sp', X2_d[t_ * 128:(t_ + 1) * 128, :], xr_, [R_xr], (), 'xrs0', accw=[R_X2d])

    xrs = [A.alloc([128, D], F32, f"xrs{i}") for i in range(1)]
    shared_expert(w_sg, w_su, w_sd)

    S.barrier()
    A.top = mark_keep
    NRING = 8
    ringb = [A.alloc([128, 4096], F32, f"ringb{i}") for i in range(NRING)]
    XeT, R_XeT = A.alloc([128, 16, 1024], BF16, "XeT")
    xe = [A.alloc([128, D], BF16, f"xe{i}") for i in range(2)]
    actSt = [A.alloc([128, 4, 128], BF16, f"actSt{i}") for i in range(2)]
    aTok, R_aTok = A.alloc([128, 8, 512], BF16, "aTok")
    ysb = [A.alloc([128, D], BF16, f"ysb{i}") for i in range(2)]
    GS2, R_GS2 = A.alloc([128, 2, 16], F32, "GS2")
    dma('sp', GS2, gate_d[2:4, :].rearrange("g (p k) -> p g k", k=16), [R_gate], [R_GS2], 'c_gs2')
    Wg_rows = w_eg.rearrange("e (p h k) f -> (e p h) (k f)", h=2, k=8)
    Wu_rows = w_eu.rearrange("e (p h k) f -> (e p h) (k f)", h=2, k=8)
    Wd_rows = w_ed.rearrange("e (p h c) d -> (e p h) (c d)", h=2, c=2)
    NRANK = int(os.environ.get('MK_NRANK', NEXP))
    cnts = {'xe': 0, 'y': 0, 'k': 0, 'w': 0}

    def wgather(rows_ap, half, r):
        i = cnts['w'] % NRING
        cnts['w'] += 1
        p_, R_p = ringb[i]
        off = bass.IndirectOffsetOnAxis(ap=idxw[:, half, r:r + 1], axis=0)
        src = rows_ap[:, :]
        S.dma('pool', lambda e: e.indirect_dma_start(out=p_, out_offset=None, in_=src, in_offset=off),
              [R_idxw], [R_p], f'ringb{i}')
        return p_, R_p

    WTS = {}

    def build_xet(r):
        C = CAPS[r]
        base = BASES[r]
        tiles = [(s0, min(128, C - s0)) for s0 in range(0, C, 128)]
        glo = wgather(Wg_rows, 0, r)
        ghi = wgather(Wg_rows, 1, r)
        ulo = wgather(Wu_rows, 0, r)
        uhi = wgather(Wu_rows, 1, r)
        dlo = wgather(Wd_rows, 0, r)
        dhi = wgather(Wd_rows, 1, r)
        WTS[r] = (glo, ghi, ulo, uhi, dlo, dhi)
        for (s0, ns) in tiles:
            xb, R_xb = xe[cnts['xe'] % 2]
            pbk = 2 * (cnts['xe'] % 2)
            cnts['xe'] += 1
            dma('sp', xb[0:ns, :], Xg_d[base + s0:base + s0 + ns, :], [R_Xg], [R_xb], f'xe{(cnts["xe"] - 1) % 2}')
            pX = bank_b(pbk, 2)
            Rp = [PB[pbk], PB[pbk + 1]]
            xv = xb[0:ns, :].rearrange("s (p k) -> s k p", k=16)
            for kc in range(16):
                tr(pX[:, kc * 128:kc * 128 + ns], xv[:, kc, :], ident_b[0:ns, 0:ns], [R_xb, R_idb], Rp)
            pv = pX.rearrange("p (k s) -> p k s", s=128)[:, :, 0:ns]
            cp('act' if cnts['xe'] % 2 == 0 else 'dve', XeT[:, :, s0:s0 + ns], pv, Rp, [R_XeT])


    def passes12(r):
        C = CAPS[r]
        base = BASES[r]
        tiles = [(s0, min(128, C - s0)) for s0 in range(0, C, 128)]
        glo, ghi, ulo, uhi, dlo, dhi = WTS[r]
        def wmov(lo, hi, kc):
            p_ = lo[0] if kc < 8 else hi[0]
            R_ = lo[1] if kc < 8 else hi[1]
            v = p_.bitcast(BF16).rearrange("p (k f two) -> p k f two", k=8, two=2)[:, kc % 8, :, 1]
            return v, R_
        for ti, (s0, ns) in enumerate(tiles):
            k_ = cnts['k']
            cnts['k'] += 1
            pbk = 4 + k_ % 2
            pg = bank_f(pbk)[0:ns, :]
            for kc in range(16):
                v, R_ = wmov(glo, ghi, kc)
                mm(pg, XeT[:, kc, s0:s0 + ns], v, kc == 0, kc == 15, [R_, R_XeT], [PB[pbk]])
            actf(aTok[0:ns, ti, :], pg, AF.Silu, [PB[pbk]], [R_aTok])
        for ti, (s0, ns) in enumerate(tiles):
            k_ = cnts['k']
            cnts['k'] += 1
            pbk = 4 + k_ % 2
            pu = bank_f(pbk)[0:ns, :]
            for kc in range(16):
                v, R_ = wmov(ulo, uhi, kc)
                mm(pu, XeT[:, kc, s0:s0 + ns], v, kc == 0, kc == 15, [R_, R_XeT], [PB[pbk]])
            tt('dve', aTok[0:ns, ti, :], pu, aTok[0:ns, ti, :], ALU.mult, [PB[pbk], R_aTok], [R_aTok])

    def passes34(r):
        C = CAPS[r]
        base = BASES[r]
        tiles = [(s0, min(128, C - s0)) for s0 in range(0, C, 128)]
        glo, ghi, ulo, uhi, dlo, dhi = WTS[r]
        for ti, (s0, ns) in enumerate(tiles):
            k_ = cnts['y']
            cnts['y'] += 1
            pbk = k_ % 2
            pT_ = bank_b(pbk)
            as_, R_as = actSt[k_ % 2]
            av = aTok[0:ns, ti, :].rearrange("s (m c) -> s c m", c=4)
            for fc in range(4):
                tr(pT_[:, fc * 128:fc * 128 + ns], av[:, fc, :], ident_b[0:ns, 0:ns], [R_aTok, R_idb], [PB[pbk]])
            cp('dve', as_[:, :, 0:ns], pT_[:, 0:512].rearrange("p (c s) -> p c s", c=4)[:, :, 0:ns], [PB[pbk]], [R_as])
            yi = k_ % 2
            y_, R_y = ysb[yi]
            for cb in range(4):
                pb = 6 + cb % 2
                cs = slice(cb * 512, (cb + 1) * 512)
                for fc in range(4):
                    p_, R_ = (dlo if fc < 2 else dhi)
                    dvv = p_.bitcast(BF16).rearrange("p (c d two) -> p c d two", c=2, two=2)[:, fc % 2, :, 1]
                    mm(bank_f(pb)[0:ns, :], as_[:, fc, 0:ns], dvv[:, cs], fc == 0, fc == 3, [R_as, R_], [PB[pb]])
                cp('act', y_[0:ns, cs], bank_f(pb)[0:ns, :], [PB[pb]], [R_y])
            dma('sp', Yg_d[base + s0:base + s0 + ns, :], y_[0:ns, :], [R_y], (), f'ysb{yi}', accw=[R_Yg])

    build_xet(0)
    for r in range(NRANK):
        passes12(r)
        if r + 1 < NRANK:
            build_xet(r + 1)
        passes34(r)

    S.barrier()
    A.top = mark_keep
    accs = [A.alloc([128, D], F32, f"accs{i}") for i in range(2)]
    ytm = [A.alloc([128, D], BF16, f"ytm{i}") for i in range(4)]
    gateB3, R_gB3 = A.alloc([128, D], F32, "gateB3")
    xr = [A.alloc([128, D], F32, f"xr{i}") for i in range(2)]
    dma('sp', gateB3, gate_d[1].partition_broadcast(128), [R_gate], [R_gB3], 'c_gB3')
    gcnt = [0]
    for t_ in range(NTT):
        ac_, R_ac = accs[t_ % 2]
        for k_ in range(8):
            i = gcnt[0] % 4
            gcnt[0] += 1
            y_, R_y = ytm[i]
            off = bass.IndirectOffsetOnAxis(ap=slots_i[:, t_, k_:k_ + 1], axis=0)
            S.dma('pool', (lambda y__, off_: lambda e: e.indirect_dma_start(out=y__, out_offset=None, in_=Yg_d[:, :],
                                                                            in_offset=off_))(y_, off),
                  [R_Yg, R_sli], [R_y], f'ytm{i}')
            if k_ == 0:
                ts('dve', ac_, y_, slw[:, t_, 1, 0:1], None, ALU.mult, None, [R_y, R_slw], [R_ac])
            else:
                stt('dve', ac_, y_, slw[:, t_, 1, k_:k_ + 1], ac_, ALU.mult, ALU.add, [R_y, R_slw, R_ac], [R_ac])
        xr_, R_xr = xr[t_ % 2]
        dma('sp', xr_, X2_d[t_ * 128:(t_ + 1) * 128, :], [R_X2d], [R_xr], f'xr{t_ % 2}')
        tt('dve', ac_, ac_, gateB3, ALU.mult, [R_ac, R_gB3], [R_ac])
        tt('pool', xr_, xr_, ac_, ALU.add, [R_xr, R_ac], [R_xr])
        dma('sp', out_d[t_ * 128:(t_ + 1) * 128, :], xr_, [R_xr], (), f'xr{t_ % 2}')

    S.emit()
    return nc


_CACHE = {}


def _ctab():
    c = np.zeros((128, 3, 64), np.float32)
    c[:, 0, :] = np.arange(64, dtype=np.float32)[None, :]
    c[:, 1, :] = np.asarray(BASES, np.float32)[None, :]
    c[:, 2, 0] = np.arange(128, dtype=np.float32)
    return np.ascontiguousarray(c.reshape(128, 192))


def _ltm():
    m = (np.arange(64)[None, :] < np.arange(64)[:, None]).astype(np.float32)
    return np.ascontiguousarray(np.tile(m.reshape(1, 4096), (128, 1)))


def _rope_tables():
    rows_n = SEQ // 64
    rows = np.repeat(np.arange(rows_n, dtype=np.float32), 64)
    cols = np.tile(np.arange(64, dtype=np.float32), rows_n)
    inv = (np.float32(10000.0) ** (-np.arange(32, dtype=np.float32) / np.float32(32))).astype(np.float32)
    ang_r = (rows[:, None] * inv).astype(np.float32)
    ang_c = (cols[:, None] * inv).astype(np.float32)
    cr, sr, cc, sc = np.cos(ang_r), np.sin(ang_r), np.cos(ang_c), np.sin(ang_c)
    cos4 = np.concatenate([cr, cr, cc, cc], axis=1)
    sin4 = np.concatenate([-sr, sr, -sc, sc], axis=1)
    lat = np.concatenate([cos4, sin4], axis=1).astype(np.float32)
    ctxp = np.concatenate([np.ones((CTX, 128), np.float32), np.zeros((CTX, 128), np.float32)], axis=1)
    return np.ascontiguousarray(np.concatenate([ctxp, lat], axis=0)), np.ascontiguousarray(lat)


def kernel(**inputs):
    NE_RUN = int(os.environ.get('MK_NEXP', 1 if os.environ.get('MK_STOP', '') else NEXP))
    f = lambda k: np.ascontiguousarray(np.asarray(inputs[k], dtype=np.float32))
    if 'nc' not in _CACHE:
        _CACHE['nc'] = build_program()
    nc = _CACHE['nc']
    rope_all, rope_lat = _rope_tables()
    x = f('x')[0]
    shared = {
        'x_all': x, 'ctx': f('ctx')[0], 'c': f('c')[0], 'c_ctx': f('c_ctx'),
        'w_ada': f('w_ada')[0], 'b_ada': f('b_ada')[0], 'norm_mix': f('norm_mix')[0], 'norm_ffn': f('norm_ffn')[0],
        'w_in': f('w_in')[0], 'q_norm_a': f('q_norm_a')[0], 'k_norm_a': f('k_norm_a')[0],
        'q_norm_b': f('q_norm_b')[0], 'k_norm_b': f('k_norm_b')[0],
        'lambda_q1': f('lambda_q1')[0], 'lambda_k1': f('lambda_k1')[0], 'lambda_q2': f('lambda_q2')[0],
        'lambda_k2': f('lambda_k2')[0], 'subln_b': f('subln_b')[0],
        'w_branch_a': f('w_branch_a')[0], 'w_branch_b': f('w_branch_b')[0], 'w_out': f('w_out')[0],
        'w_router': f('w_router')[0], 'router_bias': f('router_bias')[0],
        'w_exp_gate': f('w_exp_gate')[0][:NE_RUN], 'w_exp_up': f('w_exp_up')[0][:NE_RUN], 'w_exp_down': f('w_exp_down')[0][:NE_RUN],
        'w_sh_gate': f('w_sh_gate')[0], 'w_sh_up': f('w_sh_up')[0], 'w_sh_down': f('w_sh_down')[0],
        'ident': np.eye(128, dtype=np.float32), 'rope_all': rope_all,
        'tri': np.triu(np.ones((128, 128), np.float32), 1),
        'ctab': _ctab(), 'ltm': _ltm(),
    }
    in_maps = []
    for c in range(NCORE):
        m = dict(shared)
        m['x_own'] = np.ascontiguousarray(x[c * TOK:(c + 1) * TOK])
        m['rope_own'] = np.ascontiguousarray(rope_lat[c * TOK:(c + 1) * TOK])
        in_maps.append(m)
    res = run_bass_kernel_spmd(nc, in_maps, core_ids=list(range(NCORE)))
    out = np.concatenate([np.asarray(r['out'], dtype=np.float32) for r in res.results], axis=0)
    return out.reshape(1, SEQ, D)
```
